# Optimizing a Trainium2 kernel written in Bass

```python
import math
import jax, jax.numpy as jnp
from jax import lax
import numpy as np

D_MODEL = 1024
BATCH = 16
SEQ = 2048
DEPTH = 2

GRID_W = 64
CTX_LEN = 256
NORM_EPS = 1e-6
N_BRANCH = 4
MIX_WIDTH = D_MODEL // N_BRANCH
NEG_BIG = -1e30
F_EPS = 1e-30

A_HEAD_DIM = 64
A_HEADS = MIX_WIDTH // A_HEAD_DIM
A_CHUNK = 128
A_FGATE_BIAS_LO = 3.0
A_FGATE_BIAS_HI = 6.0

B_SHORT = 3
B_BANDS = 8
B_FEAT = 1 + 2 * B_BANDS
B_FILTER_HIDDEN = 64
B_DECAY_TARGET = 1e-2
B_FAST_DECAY = 0.3
B_SLOW_DECAY = 1.5

C_CHUNK = 128
C_GROUPS = 4

D_KEY = 64
D_HEADS = MIX_WIDTH // D_KEY
D_VAL = MIX_WIDTH // D_HEADS
D_CHUNK = 64

FFN_HIDDEN = ((-(-8 * D_MODEL // 3)) + 255) // 256 * 256

IN_COLS = 4 * MIX_WIDTH + 4 * A_HEADS + 3 * MIX_WIDTH + 2 * MIX_WIDTH + 5 * MIX_WIDTH

kernel_name = 'hybrid_mlstm_hyena_gmlp_hgrn2_dit'

F32 = jnp.float32


def rms_norm(x, w):
    xf = x.astype(F32)
    y = xf * lax.rsqrt(jnp.mean(xf * xf, axis=-1, keepdims=True) + NORM_EPS)
    return (y * w.astype(F32)).astype(x.dtype)


def layer_norm(x, w, b):
    xf = x.astype(F32)
    mu = jnp.mean(xf, axis=-1, keepdims=True)
    var = jnp.mean(jnp.square(xf - mu), axis=-1, keepdims=True)
    return ((xf - mu) * lax.rsqrt(var + NORM_EPS) * w.astype(F32) + b.astype(F32)).astype(x.dtype)


def head_rms(h, w):
    y = h * lax.rsqrt(jnp.mean(h * h, axis=-1, keepdims=True) + NORM_EPS)
    bsz, nh, n, dh = y.shape
    return y.transpose(0, 2, 1, 3).reshape(bsz, n, nh * dh) * w.astype(F32)


def modulate(h, shift, scale):
    return h * (1 + scale[:, None]) + shift[:, None]


def grid_transpose(x, rows, cols):
    bsz, n, d = x.shape
    return x.reshape(bsz, rows, cols, d).swapaxes(1, 2).reshape(bsz, n, d)


def to_heads(x, nh):
    bsz, n, ch = x.shape
    return x.reshape(bsz, n, nh, ch // nh).transpose(0, 2, 1, 3)


def to_chunks(a, size):
    s = a.shape
    return jnp.moveaxis(a.reshape(s[:2] + (s[2] // size, size) + s[3:]), 2, 0)


def from_chunks(a):
    a = jnp.moveaxis(a, 0, 2)
    s = a.shape
    return a.reshape(s[:2] + (s[2] * s[3],) + s[4:])


def split_in(p):
    sizes = (4 * MIX_WIDTH, 4 * A_HEADS, 3 * MIX_WIDTH, 2 * MIX_WIDTH)
    idx, s = [], 0
    for sz in sizes:
        s += sz
        idx.append(s)
    return jnp.split(p, idx, axis=-1)


def mlstm_chunked(q, k, v, i_pre, f_pre, state):
    n_tok, dh = q.shape[2], q.shape[3]
    size = min(A_CHUNK, n_tok)
    q = q.astype(F32)
    k = k.astype(F32) * dh ** -0.5
    v = v.astype(F32)
    log_i = i_pre.astype(F32)
    log_f = jax.nn.log_sigmoid(f_pre.astype(F32))
    tri = jnp.tril(jnp.ones((size, size), bool))

    def step(carry, blk):
        C, nrm, m = carry
        qb, kb, vb, ib, fb = blk
        b = jnp.cumsum(fb, axis=-1)
        log_d_raw = b[..., :, None] - b[..., None, :] + ib[..., None, :]
        log_inter = b + m[..., None]
        m_t = jnp.maximum(log_inter, jnp.max(jnp.where(tri, log_d_raw, NEG_BIG), axis=-1))
        d_mat = jnp.where(tri, jnp.exp(jnp.where(tri, log_d_raw - m_t[..., None], 0.0)), 0.0)
        w_intra = d_mat * jnp.einsum('bhtd,bhsd->bhts', qb, kb)
        w_inter = jnp.exp(log_inter - m_t)
        num = w_inter[..., None] * jnp.einsum('bhtk,bhkv->bhtv', qb, C) + jnp.einsum('bhts,bhsv->bhtv', w_intra, vb)
        den = w_inter * jnp.einsum('bhtk,bhk->bht', qb, nrm) + jnp.sum(w_intra, axis=-1)
        h = num / jnp.maximum(jnp.abs(den), jnp.exp(-m_t))[..., None]
        b_end = b[..., -1]
        log_w = b_end[..., None] - b + ib
        m_new = jnp.maximum(b_end + m, jnp.max(log_w, axis=-1))
        w_st = jnp.exp(log_w - m_new[..., None])
        decay = jnp.exp(b_end + m - m_new)
        C = decay[..., None, None] * C + jnp.einsum('bhs,bhsk,bhsv->bhkv', w_st, kb, vb)
        nrm = decay[..., None] * nrm + jnp.einsum('bhs,bhsk->bhk', w_st, kb)
        return (C, nrm, m_new), h

    blocks = tuple(to_chunks(a, size) for a in (q, k, v, log_i, log_f))
    final, hs = lax.scan(step, state, blocks)
    return from_chunks(hs), final


def hgrn_chunked(q, k, v, log_f, S):
    n_tok = q.shape[2]
    size = min(D_CHUNK, n_tok)
    tri = jnp.tril(jnp.ones((size, size), bool))[:, :, None]

    def step(S, blk):
        qb, kb, vb, fb = blk
        b = jnp.cumsum(fb, axis=2)
        o_inter = jnp.einsum('bhtk,bhkv->bhtv', qb * jnp.exp(b), S)
        log_a = jnp.where(tri, b[:, :, :, None, :] - b[:, :, None, :, :], 0.0)
        a_mat = jnp.where(tri, jnp.exp(log_a), 0.0)
        scores = jnp.einsum('bhtk,bhsk,bhtsk->bhts', qb, kb, a_mat)
        o = o_inter + jnp.einsum('bhts,bhsv->bhtv', scores, vb)
        b_end = b[:, :, -1]
        S = jnp.exp(b_end)[..., None] * S + jnp.einsum('bhsk,bhsv->bhkv', kb * jnp.exp(b_end[:, :, None] - b), vb)
        return S, o

    blocks = tuple(to_chunks(a.astype(F32), size) for a in (q, k, v, log_f))
    final, os_ = lax.scan(step, S, blocks)
    return from_chunks(os_), final


def bidirectional(scan_fn, fwd_in, bwd_in, init_f, init_b):
    y_f, s_f = scan_fn(*fwd_in, init_f)
    y_b, s_b = scan_fn(*[jnp.flip(a, axis=2) for a in bwd_in], init_b)
    return y_f + jnp.flip(y_b, axis=2), s_f, s_b


def mlstm_inputs(pa, pg):
    q, k, v, o = jnp.split(pa, 4, axis=-1)
    q, k, v = to_heads(q, A_HEADS), to_heads(k, A_HEADS), to_heads(v, A_HEADS)
    bsz, n, _ = pg.shape
    i_f, f_f, i_b, f_b = pg.reshape(bsz, n, 4, A_HEADS).transpose(2, 0, 3, 1)
    return (q, k, v, i_f, f_f), (q, k, v, i_b, f_b), o


def hgrn_inputs(pd, lb):
    q, i, f_f, f_b, g = jnp.split(pd, 5, axis=-1)
    q = to_heads(jax.nn.silu(q), D_HEADS)
    v = to_heads(i, D_HEADS)
    lbh = lb.reshape(D_HEADS, D_KEY)[None, :, None, :]

    def gate(fp):
        fp = to_heads(fp, D_HEADS).astype(F32)
        f = lbh + (1 - lbh) * jax.nn.sigmoid(fp)
        log_f = jnp.log(jnp.maximum(f, F_EPS))
        k = (1 - lbh) * jax.nn.sigmoid(-fp)
        return k, log_f

    k_f, lf_f = gate(f_f)
    k_b, lf_b = gate(f_b)
    return (q, k_f, v, lf_f), (q, k_b, v, lf_b), g


def short_conv(x, w, b):
    ch = x.shape[-1]
    y = lax.conv_general_dilated(x, w.astype(x.dtype)[:, None, :], window_strides=(1,), padding='SAME',
                                 dimension_numbers=('NWC', 'WIO', 'NWC'), feature_group_count=ch)
    return y + b


def hyena_kernel(L, lp):
    t = jnp.linspace(0.0, 1.0, L, dtype=F32)[:, None]
    bands = jnp.linspace(1e-4, B_BANDS - 1, B_BANDS, dtype=F32)[None]
    ang = 2 * math.pi * bands * jnp.arange(L, dtype=F32)[:, None] / L
    z = jnp.concatenate([t, jnp.cos(ang), -jnp.sin(ang)], axis=-1)
    hd = jnp.sin(lp['hy_freq1'].astype(F32) * (z @ lp['hy_w1'].astype(F32) + lp['hy_b1'].astype(F32)))
    hd = jnp.sin(lp['hy_freq2'].astype(F32) * (hd @ lp['hy_w2'].astype(F32) + lp['hy_b2'].astype(F32)))
    hk = hd @ lp['hy_w3'].astype(F32)
    deltas = jnp.abs(jnp.linspace(math.log(B_DECAY_TARGET) / B_SLOW_DECAY,
                                  math.log(B_DECAY_TARGET) / B_FAST_DECAY, MIX_WIDTH, dtype=F32))
    decay = jnp.exp(-t * deltas)
    h_fwd = hk[:, :MIX_WIDTH] * decay
    h_bwd = hk[:, MIX_WIDTH:] * decay
    return jnp.concatenate([h_fwd, jnp.zeros((1, MIX_WIDTH), F32), jnp.flip(h_bwd[1:], axis=0)], axis=0)


def long_conv(u, kernel, d_skip):
    n = u.shape[1]
    uf = jnp.fft.rfft(u.astype(F32), n=2 * n, axis=1)
    kf = jnp.fft.rfft(kernel, axis=0)
    y = jnp.fft.irfft(uf * kf[None], n=2 * n, axis=1)[:, :n]
    return (y + u.astype(F32) * d_skip.astype(F32)).astype(u.dtype)


def hyena(pb, lp):
    n = pb.shape[1]
    x0, x1, v = jnp.split(short_conv(pb, lp['hy_short_w'], lp['hy_short_b']), 3, axis=-1)
    return x0 * long_conv(x1 * v, hyena_kernel(n, lp), lp['hy_bias'])


def gmlp(pc, lp):
    u, v = jnp.split(jax.nn.gelu(pc), 2, axis=-1)
    v = layer_norm(v, lp['gm_norm_w'], lp['gm_norm_b'])
    bsz, n, _ = v.shape
    vb = v.reshape(bsz, n // C_CHUNK, C_CHUNK, C_GROUPS, MIX_WIDTH // C_GROUPS)
    mixed = jnp.einsum('gts,bnsgc->bntgc', lp['gm_ws'], vb) + lp['gm_bs'].T[None, None, :, :, None]
    return u * mixed.reshape(bsz, n, MIX_WIDTH)


def merge_branches(h, ys, lp):
    acc = jax.nn.sigmoid(h @ lp['w_gate'][0]) * (ys[0] @ lp['w_branch'][0])
    for j in range(1, N_BRANCH):
        acc = acc + jax.nn.sigmoid(h @ lp['w_gate'][j]) * (ys[j] @ lp['w_branch'][j])
    return acc @ lp['w_out']


def token_mixing(h, hc, need_ctx, lp):
    pa, pg, pb, pc, pd = split_in(h @ lp['w_in'] + lp['b_in'])
    ca, cg, cb, cc, cd = split_in(hc @ lp['w_in'] + lp['b_in'])
    bsz = h.shape[0]
    lat_f, lat_b, lat_o = mlstm_inputs(pa, pg)
    ctx_f, ctx_b, ctx_o = mlstm_inputs(ca, cg)
    zero_a = (jnp.zeros((bsz, A_HEADS, A_HEAD_DIM, A_HEAD_DIM), F32),
              jnp.zeros((bsz, A_HEADS, A_HEAD_DIM), F32), jnp.zeros((bsz, A_HEADS), F32))
    ctx_ha, sa_f, sa_b = bidirectional(mlstm_chunked, ctx_f, ctx_b, zero_a, zero_a)
    lat_ha, _, _ = bidirectional(mlstm_chunked, lat_f, lat_b, sa_f, sa_b)
    lat_f, lat_b, lat_g = hgrn_inputs(pd, lp['lb'])
    ctx_f, ctx_b, ctx_g = hgrn_inputs(cd, lp['lb'])
    zero_d = jnp.zeros((bsz, D_HEADS, D_KEY, D_VAL), F32)
    ctx_hd, sd_f, sd_b = bidirectional(hgrn_chunked, ctx_f, ctx_b, zero_d, zero_d)
    lat_hd, _, _ = bidirectional(hgrn_chunked, lat_f, lat_b, sd_f, sd_b)

    def branches(ha, o, hd, g, pb_, pc_):
        y_a = (head_rms(ha, lp['mlstm_norm_w']) * jax.nn.sigmoid(o.astype(F32))).astype(h.dtype)
        y_b = hyena(pb_, lp)
        y_c = gmlp(pc_, lp)
        y_d = (head_rms(hd, lp['hg_norm_w']) * jax.nn.silu(g.astype(F32))).astype(h.dtype)
        return (y_a, y_b, y_c, y_d)

    y = merge_branches(h, branches(lat_ha, lat_o, lat_hd, lat_g, pb, pc), lp)
    yc = merge_branches(hc, branches(ctx_ha, ctx_o, ctx_hd, ctx_g, cb, cc), lp) if need_ctx else None
    return y, yc


def swiglu(h, w_up, w_down):
    a, g = jnp.split(h @ w_up, 2, axis=-1)
    return (jax.nn.silu(a) * g) @ w_down


def setup_inputs(seed: int = 0) -> dict:
    key = jax.random.key(seed)
    keys = jax.random.split(key, 48)
    counter = [0]

    def nrm(shape, scale):
        k = keys[counter[0]]
        counter[0] += 1
        return jax.random.normal(k, shape, jnp.float32) * scale

    D = D_MODEL
    x = nrm((BATCH, SEQ, D), 1.0)
    c = nrm((BATCH, D), 1.0)
    ctx = nrm((BATCH, CTX_LEN, D), 1.0)
    c_ctx = nrm((D,), 1.0)
    ada_w = nrm((DEPTH, D, 6 * D), D ** -0.5)
    ada_b = nrm((DEPTH, 6 * D), 0.02)
    norm1_w = 1.0 + nrm((DEPTH, D), 0.1)
    norm2_w = 1.0 + nrm((DEPTH, D), 0.1)
    w_in = nrm((DEPTH, D, IN_COLS), D ** -0.5)
    b_in = nrm((DEPTH, IN_COLS), 0.02)
    off = 4 * MIX_WIDTH
    f_bias = jnp.linspace(A_FGATE_BIAS_LO, A_FGATE_BIAS_HI, A_HEADS, dtype=jnp.float32)
    b_in = b_in.at[:, off + A_HEADS: off + 2 * A_HEADS].add(f_bias)
    b_in = b_in.at[:, off + 3 * A_HEADS: off + 4 * A_HEADS].add(f_bias)
    mlstm_norm_w = 1.0 + nrm((DEPTH, MIX_WIDTH), 0.1)
    hy_short_w = nrm((DEPTH, B_SHORT, 3 * MIX_WIDTH), B_SHORT ** -0.5)
    hy_short_b = nrm((DEPTH, 3 * MIX_WIDTH), 0.02)
    hy_w1 = nrm((DEPTH, B_FEAT, B_FILTER_HIDDEN), B_FEAT ** -0.5)
    hy_b1 = nrm((DEPTH, B_FILTER_HIDDEN), 0.1)
    hy_freq1 = 1.0 + nrm((DEPTH, B_FILTER_HIDDEN), 0.1)
    hy_w2 = nrm((DEPTH, B_FILTER_HIDDEN, B_FILTER_HIDDEN), B_FILTER_HIDDEN ** -0.5)
    hy_b2 = nrm((DEPTH, B_FILTER_HIDDEN), 0.1)
    hy_freq2 = 1.0 + nrm((DEPTH, B_FILTER_HIDDEN), 0.1)
    hy_w3 = nrm((DEPTH, B_FILTER_HIDDEN, 2 * MIX_WIDTH), 0.05 * B_FILTER_HIDDEN ** -0.5)
    hy_bias = nrm((DEPTH, MIX_WIDTH), 0.5)
    gm_norm_w = 1.0 + nrm((DEPTH, MIX_WIDTH), 0.1)
    gm_norm_b = nrm((DEPTH, MIX_WIDTH), 0.02)
    gm_ws = nrm((DEPTH, C_GROUPS, C_CHUNK, C_CHUNK), C_CHUNK ** -0.5)
    gm_bs = 1.0 + nrm((DEPTH, C_GROUPS, C_CHUNK), 0.1)
    hg_lb_logits = nrm((DEPTH, MIX_WIDTH), 0.5)
    hg_norm_w = 1.0 + nrm((DEPTH, MIX_WIDTH), 0.1)
    w_gate = nrm((DEPTH, N_BRANCH, D, D), D ** -0.5)
    w_branch = nrm((DEPTH, N_BRANCH, MIX_WIDTH, D), MIX_WIDTH ** -0.5)
    w_out = nrm((DEPTH, D, D), D ** -0.5)
    w_ffn_in = nrm((DEPTH, D, 2 * FFN_HIDDEN), D ** -0.5)
    w_ffn_out = nrm((DEPTH, FFN_HIDDEN, D), FFN_HIDDEN ** -0.5)
    final_norm_w = 1.0 + nrm((D,), 0.1)
    return {'x': x, 'c': c, 'ctx': ctx, 'c_ctx': c_ctx, 'ada_w': ada_w, 'ada_b': ada_b,
            'norm1_w': norm1_w, 'norm2_w': norm2_w, 'w_in': w_in, 'b_in': b_in, 'mlstm_norm_w': mlstm_norm_w,
            'hy_short_w': hy_short_w, 'hy_short_b': hy_short_b, 'hy_w1': hy_w1, 'hy_b1': hy_b1,
            'hy_freq1': hy_freq1, 'hy_w2': hy_w2, 'hy_b2': hy_b2, 'hy_freq2': hy_freq2, 'hy_w3': hy_w3,
            'hy_bias': hy_bias, 'gm_norm_w': gm_norm_w, 'gm_norm_b': gm_norm_b, 'gm_ws': gm_ws, 'gm_bs': gm_bs,
            'hg_lb_logits': hg_lb_logits, 'hg_norm_w': hg_norm_w, 'w_gate': w_gate, 'w_branch': w_branch,
            'w_out': w_out, 'w_ffn_in': w_ffn_in, 'w_ffn_out': w_ffn_out, 'final_norm_w': final_norm_w}


def reference(x, c, ctx, c_ctx, ada_w, ada_b, norm1_w, norm2_w, w_in, b_in, mlstm_norm_w,
              hy_short_w, hy_short_b, hy_w1, hy_b1, hy_freq1, hy_w2, hy_b2, hy_freq2, hy_w3,
              hy_bias, gm_norm_w, gm_norm_b, gm_ws, gm_bs, hg_lb_logits, hg_norm_w, w_gate, w_branch,
              w_out, w_ffn_in, w_ffn_out, final_norm_w):
    rows = x.shape[1] // GRID_W
    lb_prob = jax.nn.softmax(hg_lb_logits.astype(F32), axis=0)
    lb_all = jnp.cumsum(lb_prob, axis=0) - lb_prob[0]
    sc = jax.nn.silu(c)
    scc = jax.nn.silu(c_ctx)[None]
    xc = ctx
    for l in range(DEPTH):
        need_ctx = l < DEPTH - 1
        col_major = l % 2 == 1
        if col_major:
            x = grid_transpose(x, rows, GRID_W)
        lp = {'w_in': w_in[l], 'b_in': b_in[l], 'mlstm_norm_w': mlstm_norm_w[l],
              'hy_short_w': hy_short_w[l], 'hy_short_b': hy_short_b[l], 'hy_w1': hy_w1[l], 'hy_b1': hy_b1[l],
              'hy_freq1': hy_freq1[l], 'hy_w2': hy_w2[l], 'hy_b2': hy_b2[l], 'hy_freq2': hy_freq2[l],
              'hy_w3': hy_w3[l], 'hy_bias': hy_bias[l], 'gm_norm_w': gm_norm_w[l], 'gm_norm_b': gm_norm_b[l],
              'gm_ws': gm_ws[l], 'gm_bs': gm_bs[l], 'lb': lb_all[l], 'hg_norm_w': hg_norm_w[l],
              'w_gate': w_gate[l], 'w_branch': w_branch[l], 'w_out': w_out[l]}
        mod = jnp.split(sc @ ada_w[l] + ada_b[l], 6, axis=-1)
        mod_c = jnp.split(scc @ ada_w[l] + ada_b[l], 6, axis=-1)
        h = modulate(rms_norm(x, norm1_w[l]), mod[0], mod[1])
        hc = modulate(rms_norm(xc, norm1_w[l]), mod_c[0], mod_c[1])
        y, yc = token_mixing(h, hc, need_ctx, lp)
        x = x + mod[2][:, None] * y
        x = x + mod[5][:, None] * swiglu(modulate(rms_norm(x, norm2_w[l]), mod[3], mod[4]), w_ffn_in[l], w_ffn_out[l])
        if need_ctx:
            xc = xc + mod_c[2][:, None] * yc
            xc = xc + mod_c[5][:, None] * swiglu(modulate(rms_norm(xc, norm2_w[l]), mod_c[3], mod_c[4]),
                                                 w_ffn_in[l], w_ffn_out[l])
        if col_major:
            x = grid_transpose(x, GRID_W, rows)
    return rms_norm(x, final_norm_w)
```

```python
import math, contextlib
import numpy as np
import ml_dtypes
import concourse.bass as bass
import concourse.mybir as mybir
from concourse.bass_utils import run_bass_kernel_spmd

F32 = mybir.dt.float32
BF16 = mybir.dt.bfloat16
AF = mybir.ActivationFunctionType
ALU = mybir.AluOpType

ENGS = ("pe", "act", "dve", "pool", "sp")
SEM_EPOCH = 20000

D = 1024
KC = 8
SEQ = 2048
CTXL = 256
T = SEQ + CTXL
NT = T // 128
BLOCKS = [(0, 256), (256, 512), (768, 512), (1280, 512), (1792, 512)]
FH = 2816
FCH = 22
EPS = 1e-6
PI = math.pi


class Res:
    __slots__ = ("name", "parent", "children", "w", "rs", "dma_sem", "dma_cnt")

    def __init__(self, name, parent=None):
        self.name = name
        self.parent = parent
        self.children = {}
        self.w = None
        self.rs = []
        self.dma_sem = None
        self.dma_cnt = 0

    def sub(self, key):
        r = self.children.get(key)
        if r is None:
            r = Res(f"{self.name}/{key}", self)
            self.children[key] = r
        return r

    def family(self):
        out = [self]
        p = self.parent
        while p is not None:
            out.append(p)
            p = p.parent
        stack = list(self.children.values())
        while stack:
            c = stack.pop()
            out.append(c)
            stack.extend(c.children.values())
        return out


class Rec:
    def __init__(self, nc):
        self.nc = nc
        self.ops = {e: [] for e in ENGS}
        self.cnt = {e: 0 for e in ENGS}
        self.seen = {e: {} for e in ENGS}
        self.semkeys = []
        self.semset = set()
        self.dma_res = []
        self.n_dma_sem = 0
        self.tags = {}
        self.free_sems = {}
        self.dcount = {}

    def _sem(self, key):
        if key not in self.semset:
            self.semset.add(key)
            self.semkeys.append(key)
        return key

    def _need(self, eng, dep, waits):
        if dep is None:
            return
        key, val, deng = dep
        if self.seen[eng].get(key, 0) >= val:
            return
        self.seen[eng][key] = val
        waits.append((key, val))

    def op(self, eng, fn, reads=(), writes=(), dma=False):
        waits = []
        for r in reads:
            for f in r.family():
                if f.w is not None:
                    if f.w[2] == eng and eng == "pe" and not dma and f.w[0][0] == "e":
                        continue
                    self._need(eng, f.w, waits)
        for w in writes:
            for f in w.family():
                if f.w is not None and not (f.w[2] == eng and eng == "pe" and not dma and f.w[0][0] == "e"):
                    self._need(eng, f.w, waits)
                for rd in f.rs:
                    if rd[2] == eng and eng == "pe" and not dma and rd[0][0] == "e":
                        continue
                    self._need(eng, rd, waits)
        if dma:
            dst = writes[0]
            if dst.dma_sem is None:
                fl = self.free_sems.setdefault(eng, [])
                if fl:
                    dst.dma_sem = fl.pop()
                else:
                    dst.dma_sem = self._sem(("d", eng, self.n_dma_sem))
                    self.n_dma_sem += 1
            assert dst.dma_sem[1] == eng, f"DMA semaphore of {dst.name} shared across queues"
            cntv = self.dcount.get(dst.dma_sem, 0) + 16
            self.dcount[dst.dma_sem] = cntv
            dst.dma_cnt = cntv
            done = (dst.dma_sem, cntv, eng)
            inc = (dst.dma_sem, 16)
        else:
            n = self.cnt[eng]
            self.cnt[eng] = n + 1
            ep = n // SEM_EPOCH
            key = self._sem(("e", eng, ep))
            done = (key, n - ep * SEM_EPOCH + 1, eng)
            inc = (key, 1)
        for w in writes:
            w.w = done
            w.rs = []
            stack = list(w.children.values())
            while stack:
                c = stack.pop()
                c.w = done
                c.rs = []
                stack.extend(c.children.values())
        for r in reads:
            r.rs.append(done)
            if len(r.rs) > 48:
                r.rs = r.rs[-48:]
        import sys as _sys
        fr = _sys._getframe(2)
        tag = f"{fr.f_code.co_name}:{fr.f_lineno}"
        fr2 = fr.f_back
        if fr2 is not None:
            tag += f"<{fr2.f_code.co_name}:{fr2.f_lineno}"
        self.ops[eng].append((waits, fn, inc, tag))
        return done

    def free_res(self, res):
        stack = [res]
        while stack:
            r = stack.pop()
            if r.dma_sem is not None:
                self.free_sems.setdefault(r.dma_sem[1], []).append(r.dma_sem)
                r.dma_sem = None
            stack.extend(r.children.values())

    def barrier(self):
        deps = []
        for e in ENGS:
            n = self.cnt[e]
            if n > 0:
                ep = (n - 1) // SEM_EPOCH
                deps.append((("e", e, ep), n - ep * SEM_EPOCH, e))
        for key, cntv in self.dcount.items():
            deps.append((key, cntv, "x"))
        for e in ENGS:
            waits = []
            for d in deps:
                self._need(e, d, waits)
            if waits:
                self.ops[e].append((waits, None, None, "barrier"))

    def emit(self):
        nc = self.nc
        with contextlib.ExitStack() as st:
            sems = {}
            for k in self.semkeys:
                sems[k] = st.enter_context(nc.semaphore("s_" + "_".join(str(x) for x in k)))
            block = st.enter_context(nc.Block())
            emap = {"pe": block.tensor, "act": block.scalar, "dve": block.vector,
                    "pool": block.gpsimd, "sp": block.sync}
            for e in ENGS:
                ops = self.ops[e]
                if not ops:
                    continue

                def body(eng, ops=ops):
                    for waits, fn, inc, tag in ops:
                        for (k, v) in waits:
                            eng.wait_ge(sems[k], v)
                        if fn is not None:
                            ins = fn(eng)
                            try:
                                self.tags[str(ins.ins.name)] = tag + ' | ' + str(ins.ins.concise())[:300]
                            except Exception:
                                pass
                            ins.then_inc(sems[inc[0]], inc[1])
                emap[e](body)


def _col(v, P=128):
    v = np.asarray(v, np.float32)
    return np.ascontiguousarray(v.reshape(-1, P).T)


class Pack:
    def __init__(self):
        self.items = []
        self.off = {}
        self.n = 0

    def add(self, name, arr):
        arr = np.asarray(arr, np.float32)
        if arr.ndim == 1:
            arr = arr[:, None]
        arr = arr.reshape(arr.shape[0], -1)
        self.off[name] = (self.n, arr.shape[0], arr.shape[1])
        self.items.append(arr)
        self.n += arr.shape[1]

    def build(self):
        out = np.zeros((128, self.n), np.float32)
        for (name, (o, p, c)), a in zip(self.off.items(), self.items):
            out[:p, o:o + c] = a
        return out


def _dft_tables(L):
    N = 2 * L
    m = np.arange(L, dtype=np.float64)[:, None]
    f = np.arange(L, dtype=np.float64)[None, :]
    ang = 2 * np.pi * ((m * f) % N) / N
    Fc = np.cos(ang)
    Fs = np.sin(ang)
    Fs[:, 0] = np.cos(np.pi * m[:, 0])
    F = np.concatenate([Fc, Fs], axis=1)
    wc = np.full((L, 1), 2.0 / N)
    wc[0, 0] = 1.0 / N
    Gc = wc * np.cos(ang.T)
    Gs = -(2.0 / N) * np.sin(ang.T)
    Gs[0, :] = np.cos(np.pi * m[:, 0]) / N
    G = np.concatenate([Gc, Gs], axis=0)
    return F.astype(np.float32), G.astype(np.float32)


def _hy_tables(L):
    t = np.linspace(0.0, 1.0, L, dtype=np.float32)[:, None]
    bands = np.linspace(1e-4, 7, 8, dtype=np.float32)[None]
    ang = (2 * np.float32(math.pi) * bands * np.arange(L, dtype=np.float32)[:, None] / np.float32(L)).astype(np.float32)
    z = np.concatenate([t, np.cos(ang), -np.sin(ang)], axis=-1).astype(np.float32)
    deltas = np.abs(np.linspace(math.log(1e-2) / 1.5, math.log(1e-2) / 0.3, 256, dtype=np.float32))
    decay = np.exp(-t * deltas[None]).astype(np.float32)
    decay0 = decay.copy()
    decay0[0] = 0.0
    return z, decay, decay0


_CONST_CACHE = {}


def _constants():
    if _CONST_CACHE:
        return _CONST_CACHE
    bf = ml_dtypes.bfloat16
    c = {}
    idx = np.arange(128)
    tri = (idx[:, None] <= idx[None, :]).astype(np.float32)
    ident = np.eye(128, dtype=np.float32)
    selrow = np.zeros((128, 4, 64), np.float32)
    for h in range(4):
        selrow[h, h, :] = 1.0
    pick = np.zeros((128, 2, 64), np.float32)
    pick[127, 0, :] = 1.0
    pick[0, 1, :] = 1.0
    c["cst_f"] = np.ascontiguousarray(np.concatenate([ident, tri, tri.T.copy(), selrow.reshape(128, 256), pick.reshape(128, 128)], axis=1))
    c["cst_b"] = np.ascontiguousarray(np.concatenate([ident, tri, tri.T.copy(), np.ones((128, 128), np.float32)], axis=1).astype(bf))
    rm = np.ones((64, 2, T), np.float32)
    rm[:, 0, 0::128] = 0.0
    rm[:, 1, 127::128] = 0.0
    c["rmask"] = rm.astype(bf)
    for L, tag in ((SEQ, "L"), (CTXL, "S")):
        F, G = _dft_tables(L)
        mc, fc = L // 128, (2 * L) // 128
        c["F" + tag] = np.ascontiguousarray(F.reshape(mc, 128, fc, 128).transpose(2, 1, 0, 3)).astype(bf)
        n = min(512, L)
        c["G" + tag] = np.ascontiguousarray(G.reshape(fc, 128, L // n, n).transpose(2, 1, 0, 3)).astype(bf)
        z, decay, decay0 = _hy_tables(L)
        c["z" + tag] = np.ascontiguousarray(z.T)
        dd = np.stack([decay, decay0], axis=1)
        c["dec" + tag] = np.ascontiguousarray(dd.reshape(mc, 128, 2, 256).transpose(1, 0, 2, 3))
    _CONST_CACHE.update(c)
    return c


def _prep_shared(inp):
    P = Pack()
    L2 = 2
    P.add("ada_bT", np.stack([_col(inp["ada_b"][l]) for l in range(L2)], 1))
    P.add("n1T", np.stack([_col(inp["norm1_w"][l]) for l in range(L2)], 1))
    P.add("n2T", np.stack([_col(inp["norm2_w"][l]) for l in range(L2)], 1))
    P.add("fnT", _col(inp["final_norm_w"]))
    b_in = inp["b_in"]
    P.add("bA", np.stack([_col(b_in[l, 0:1024], 64) for l in range(L2)], 1))
    P.add("bGrow", np.broadcast_to(b_in[None, :, 1024:1040], (128, 2, 16)))
    P.add("bB", np.stack([_col(b_in[l, 1040:1808]) for l in range(L2)], 1))
    P.add("bC", np.stack([_col(b_in[l, 1808:2320], 64) for l in range(L2)], 1))
    P.add("bD", np.stack([_col(b_in[l, 2320:3600], 64) for l in range(L2)], 1))
    P.add("mnw", np.stack([_col(inp["mlstm_norm_w"][l], 64) for l in range(L2)], 1))
    P.add("hnw", np.stack([_col(inp["hg_norm_w"][l], 64) for l in range(L2)], 1))
    P.add("gnw", np.stack([_col(inp["gm_norm_w"][l], 64) for l in range(L2)], 1))
    P.add("gnb", np.stack([_col(inp["gm_norm_b"][l], 64) for l in range(L2)], 1))
    P.add("lbz", np.stack([_col(inp["hg_lb_logits"][l], 64) for l in range(L2)], 1))
    P.add("hsw", np.stack([np.stack([_col(inp["hy_short_w"][l, j]) for j in range(3)], 1) for l in range(L2)], 1))
    P.add("hsb", np.stack([_col(inp["hy_short_b"][l]) for l in range(L2)], 1))
    P.add("hskip", np.stack([_col(inp["hy_bias"][l]) for l in range(L2)], 1))
    P.add("hw1", np.stack([inp["hy_w1"][l] for l in range(L2)], 1))
    P.add("hw2", np.stack([inp["hy_w2"][l] for l in range(L2)], 1))
    P.add("hw3", np.stack([inp["hy_w3"][l] for l in range(L2)], 1))
    for nm in ("hy_b1", "hy_freq1", "hy_b2", "hy_freq2"):
        P.add(nm, np.stack([inp[nm][l] for l in range(L2)], 1))
    return P


def _bf(a):
    return np.ascontiguousarray(np.asarray(a, np.float32).astype(ml_dtypes.bfloat16))


class Prog:
    def __init__(self, NB=2, NL=2, stages="ABCD", dbg=False):
        self.NB, self.NL, self.stages, self.dbg = NB, NL, stages, dbg
        self.nc = bass.Bass("TRN2", target_bir_lowering=False)
        self.R = Rec(self.nc)
        self.dram = {}
        self.dres = {}
        self.top = 0
        self.psi = 0
        self.psbi = 0
        self.dbg_outs = []

    def din(self, name, shape, dt=F32):
        self.dram[name] = self.nc.dram_tensor(name, list(shape), dt, kind="ExternalInput").ap()
        return self.dram[name]

    def dscratch(self, name, shape, dt=F32):
        self.dram[name] = self.nc.dram_tensor(name, list(shape), dt, kind="Internal").ap()
        self.dres[name] = Res(name)
        return self.dram[name]

    def alloc(self, name, P, free, dt=F32, hi=False):
        n = int(np.prod(free))
        words = n if dt == F32 else (n + 1) // 2
        words = (words + 7) // 8 * 8
        if not hasattr(self, "hi"):
            self.hi = self.AW
        assert self.top + words <= self.hi, f"SBUF arena overflow at {name}: {self.top}+{words}>{self.hi}"
        if hi:
            self.hi -= words
            base = self.hi
        else:
            base = self.top
        sl = self.arena[0:P, base:base + words]
        if dt != F32:
            sl = sl.bitcast(dt)
        sl = sl[:, 0:n]
        if len(free) == 2:
            sl = sl.rearrange("p (a b) -> p a b", a=free[0], b=free[1])
        elif len(free) == 3:
            sl = sl.rearrange("p (a b c) -> p a b c", a=free[0], b=free[1], c=free[2])
        if not hasattr(self, "alloc_log"):
            self.alloc_log = []
        _res = Res(name)
        self.alloc_log.append((base, hi, _res))
        if not hi:
            self.top += words
        self.peak = max(getattr(self, "peak", 0), self.top + (self.AW - self.hi))
        return sl, _res

    def mark(self):
        return self.top

    def release(self, m, hi=False):
        self.R.barrier()
        self.top = m
        if hi:
            self.hi = self.AW
        keep = []
        for (base, ishi, r) in getattr(self, "alloc_log", []):
            dead = (base >= self.hi) is False and ishi and hi
            if ishi:
                dead = hi
            else:
                dead = base >= m
            if dead:
                self.R.free_res(r)
            else:
                keep.append((base, ishi, r))
        self.alloc_log = keep

    def PS(self):
        i = self.psi
        self.psi = (i + 1) % len(self.psf)
        return self.psf[i], self.psf_res[i]

    def PSB(self):
        i = self.psbi
        self.psbi = (i + 1) % len(self.psb)
        return self.psb[i], self.psb_res[i]

    def mm(self, out, lhsT, rhs, start=True, stop=True, rd=(), wr=()):
        self.R.op("pe", lambda e: e.matmul(out, lhsT=lhsT, rhs=rhs, start=start, stop=stop), reads=rd, writes=wr)

    def tr(self, out, in_, ident, rd=(), wr=()):
        self.R.op("pe", lambda e: e.transpose(out=out, in_=in_, identity=ident), reads=rd, writes=wr)

    def act(self, out, in_, func, rd=(), wr=(), bias=None, scale=None):
        kw = {}
        if bias is not None:
            kw["bias"] = bias
        if scale is not None:
            kw["scale"] = scale
        self.R.op("act", lambda e: e.activation(out=out, in_=in_, func=func, **kw), reads=rd, writes=wr)

    def tt(self, out, a, b, op, rd=(), wr=(), eng="dve"):
        self.R.op(eng, lambda e: e.tensor_tensor(out=out, in0=a, in1=b, op=op), reads=rd, writes=wr)

    def ts(self, out, a, s1, op0, rd=(), wr=(), s2=None, op1=None, eng="dve"):
        if op1 is None:
            self.R.op(eng, lambda e: e.tensor_scalar(out=out, in0=a, scalar1=s1, scalar2=None, op0=op0), reads=rd, writes=wr)
        else:
            self.R.op(eng, lambda e: e.tensor_scalar(out=out, in0=a, scalar1=s1, scalar2=s2, op0=op0, op1=op1), reads=rd, writes=wr)

    def stt(self, out, a, s, b, op0, op1, rd=(), wr=()):
        self.R.op("dve", lambda e: e.scalar_tensor_tensor(out=out, in0=a, scalar=s, in1=b, op0=op0, op1=op1), reads=rd, writes=wr)

    def cp(self, out, in_, rd=(), wr=(), eng="dve"):
        self.R.op(eng, lambda e: e.tensor_copy(out=out, in_=in_), reads=rd, writes=wr)

    def recip(self, out, in_, rd=(), wr=()):
        self.R.op("dve", lambda e: e.reciprocal(out=out, in_=in_), reads=rd, writes=wr)

    def memset(self, ap, val, wr=(), eng="dve"):
        self.R.op(eng, lambda e: e.memset(ap, val), writes=wr)

    def scan(self, out, d0, d1, init, op0, op1, rd=(), wr=()):
        self.R.op("dve", lambda e: e.tensor_tensor_scan(out=out, data0=d0, data1=d1, initial=init, op0=op0, op1=op1), reads=rd, writes=wr)

    def dma(self, eng, out, in_, rd=(), wr=()):
        self.R.op(eng, lambda e: e.dma_start(out=out, in_=in_), reads=rd, writes=wr, dma=True)

    def dbg_dump(self, name, ap, res, shape, dt=F32):
        if not self.dbg:
            return
        o = self.nc.dram_tensor(name, list(shape), dt, kind="ExternalOutput").ap()
        self.dbg_outs.append(name)
        self.dma("sp", o, ap, rd=[res], wr=[Res("dbg_" + name)])

    def build(self, prm_off, NPRM):
        nc = self.nc
        NB, NL = self.NB, self.NL
        self.prm_off = prm_off
        xin = self.din("xin", [NB, SEQ, D])
        cin = self.din("cin", [NB, CTXL, D])
        self.din("csT", [128, KC, 3])
        self.din("prm", [128, NPRM])
        self.din("ada_w", [2, D, 6 * D])
        self.din("w_in", [2, D, 3600])
        self.din("w_gate", [2, 4, D, D])
        self.din("w_branch", [2, 4, 256, D])
        self.din("w_out", [2, D, D])
        self.din("w_ffn_in", [2, D, 2 * FH])
        self.din("w_ffn_out", [2, FH, D])
        self.din("gwsT", [128, 2, 4, 128])
        self.din("gbs", [1, 1024])
        self.din("cst_f", [128, 768])
        self.din("cst_b", [128, 512], BF16)
        self.din("rmask", [64, 2, T], BF16)
        for tag, L in (("L", SEQ), ("S", CTXL)):
            mc, fc = L // 128, 2 * L // 128
            n = min(512, L)
            self.din("F" + tag, [fc, 128, mc, 128], BF16)
            self.din("G" + tag, [L // n, 128, fc, n], BF16)
            self.din("z" + tag, [17, L])
            self.din("dec" + tag, [128, mc, 2, 256])
        yout = self.nc.dram_tensor("yout", [NB, SEQ, D], F32, kind="ExternalOutput").ap()
        self.yout = yout
        self.yres = Res("yout")
        park = self.dscratch("park", [128, KC, T])
        self.park, self.park_res = park, self.dres["park"]

        with contextlib.ExitStack() as st:
            self.AW = 53184
            self.arena = st.enter_context(nc.sbuf_tensor("arena", [128, self.AW], F32))
            self.psf = [st.enter_context(nc.psum_tensor(f"psf{i}", [128, 512], F32)) for i in range(6)]
            self.psb = [st.enter_context(nc.psum_tensor(f"psb{i}", [128, 1024], BF16)) for i in range(2)]
            self.psf_res = [Res(f"psf{i}") for i in range(6)]
            self.psb_res = [Res(f"psb{i}") for i in range(2)]
            self.body()
            self.R.barrier()
            self.R.emit()
        return nc

    def P(self, name):
        o, p, c = self.prm_off[name]
        return self.prm[0:p, o:o + c]

    def body(self):
        NB, NL = self.NB, self.NL
        d = self.dram
        self.prm, self.prm_r = self.alloc("prm", 128, [self.prm_off["__n__"]])
        self.dma("sp", self.prm, d["prm"], wr=[self.prm_r])
        cf, self.cf_r = self.alloc("cst_f", 128, [768])
        self.dma("sp", cf, d["cst_f"], wr=[self.cf_r])
        cb, self.cb_r = self.alloc("cst_b", 128, [512], BF16)
        self.dma("sp", cb, d["cst_b"], wr=[self.cb_r])
        self.ident_f, self.triF, self.triB = cf[:, 0:128], cf[:, 128:256], cf[:, 256:384]
        self.selrow = cf[0:4, 384:640].rearrange("p (h c) -> p h c", h=4, c=64)
        self.pick_last, self.pick_first = cf[:, 640:704], cf[:, 704:768]
        self.ident_b, self.maskF, self.maskB, self.ones_b = cb[:, 0:128], cb[:, 128:256], cb[:, 256:384], cb[:, 384:512]
        self.CR = [self.cf_r, self.cb_r, self.prm_r]
        cc, self.cc_r = self.alloc("consts", 128, [8])
        self.memset(cc[:, 0:1], EPS, wr=[self.cc_r])
        self.memset(cc[:, 1:2], -math.log(8.0), wr=[self.cc_r])
        self.memset(cc[:, 2:3], 1.0, wr=[self.cc_r])
        self.memset(cc[:, 3:4], 0.0, wr=[self.cc_r])
        self.c_eps, self.c_nl8, self.c_one, self.c_zero = cc[:, 0:1], cc[:, 1:2], cc[:, 2:3], cc[:, 3:4]
        self.CR.append(self.cc_r)
        for i in range(6):
            self.memset(self.psf[i][:, :], 0.0, wr=[self.psf_res[i]])
        self.mods()
        base = self.mark()
        for bi in range(NB):
            self.load_x(bi)
            for l in range(NL):
                self.layer(bi, l)
                if l == 0 and NL > 1:
                    self.permute(to_col=True)
            self.final(bi)
            self.release(base)

    def mods(self):
        d = self.dram
        NL = self.NL
        self.mod, self.mod_r = self.alloc("mod", 128, [2, 3, 48])
        self.drv, self.drv_r = self.alloc("drv", 128, [2, 3, 16])
        self.lb, self.lb_r = self.alloc("lb", 64, [2, 2, 4])
        m0 = self.mark()
        cs, cs_r = self.alloc("csS", 128, [KC, 3])
        self.dma("sp", cs, d["csT"], wr=[cs_r])
        self.act(cs, cs, AF.Silu, rd=[cs_r], wr=[cs_r])
        abuf = [self.alloc(f"adaw{i}", 128, [KC, 512]) for i in range(2)]
        for l in range(NL):
            ps, ps_r = self.PS()
            for ct in range(12):
                ab, ab_r = abuf[ct % 2]
                self.dma("sp", ab, d["ada_w"][l, :, ct * 512:(ct + 1) * 512].rearrange("(k p) c -> p k c", p=128), wr=[ab_r])
                for c4 in range(4):
                    c = ct * 4 + c4
                    for kc in range(KC):
                        self.mm(ps[:, c * 3:(c + 1) * 3], ab[:, kc, c4 * 128:(c4 + 1) * 128], cs[:, kc, :],
                                start=(kc == 0), stop=(kc == KC - 1), rd=[ab_r, cs_r], wr=[ps_r])
            abT = self.P("ada_bT").rearrange("p (l c) -> p l c", l=2, c=48)
            psv = ps[:, 0:144].rearrange("p (c j) -> p j c", c=48, j=3)
            for j in range(3):
                self.tt(self.mod[:, l, j, :], psv[:, j, :], abT[:, l, :], ALU.add, rd=[ps_r, self.prm_r], wr=[self.mod_r])
            n1 = self.P("n1T").rearrange("p (l c) -> p l c", l=2, c=8)
            n2 = self.P("n2T").rearrange("p (l c) -> p l c", l=2, c=8)
            for j in range(3):
                self.stt(self.drv[:, l, j, 0:8], self.mod[:, l, j, 8:16], 1.0, n1[:, l, :], ALU.add, ALU.mult,
                         rd=[self.mod_r, self.prm_r], wr=[self.drv_r])
                self.stt(self.drv[:, l, j, 8:16], self.mod[:, l, j, 32:40], 1.0, n2[:, l, :], ALU.add, ALU.mult,
                         rd=[self.mod_r, self.prm_r], wr=[self.drv_r])
        lz = self.P("lbz").rearrange("p (l h) -> p l h", l=2, h=4)
        self.memset(self.lb[:, 0, 0, :], 0.0, wr=[self.lb_r])
        self.memset(self.lb[:, 0, 1, :], 1.0, wr=[self.lb_r])
        self.tt(self.lb[:, 1, 0, :], lz[:, 1, :], lz[:, 0, :], ALU.subtract, rd=[self.prm_r], wr=[self.lb_r])
        self.act(self.lb[:, 1, 0, :], self.lb[:, 1, 0, :], AF.Sigmoid, rd=[self.lb_r], wr=[self.lb_r])
        self.ts(self.lb[:, 1, 1, :], self.lb[:, 1, 0, :], -1.0, ALU.mult, s2=1.0, op1=ALU.add, rd=[self.lb_r], wr=[self.lb_r])
        self.CR += [self.mod_r, self.drv_r, self.lb_r]
        self.dbg_dump("dbg_mod", self.mod[:, 0:NL], self.mod_r, [128, NL, 3, 48])
        self.release(m0)

    def load_x(self, bi):
        d = self.dram
        m = self.mark()
        stg = [self.alloc(f"xstg{i}", 128, [D]) for i in range(2)]
        xs = [self.alloc(f"xs{i}", 128, [KC, 128]) for i in range(2)]
        for t in range(NT):
            sb, sb_r = stg[t % 2]
            src = d["cin"][bi, t * 128:(t + 1) * 128, :] if t < 2 else d["xin"][bi, (t - 2) * 128:(t - 1) * 128, :]
            self.dma("sp", sb, src, wr=[sb_r])
            xo, xo_r = xs[t % 2]
            for half in range(2):
                ps, ps_r = self.PS()
                for q in range(4):
                    kc = half * 4 + q
                    self.tr(ps[:, q * 128:(q + 1) * 128], sb[:, kc * 128:(kc + 1) * 128], self.ident_f, rd=[sb_r, self.cf_r], wr=[ps_r])
                self.cp(xo[:, half * 4:(half + 1) * 4, :], ps[:, :].rearrange("p (k t) -> p k t", k=4, t=128), rd=[ps_r], wr=[xo_r],
                        eng="dve")
            self.dma("sp", self.park[:, :, t * 128:(t + 1) * 128], xo, rd=[xo_r], wr=[self.park_res.sub(t)])
        self.release(m)

    def park_subs(self, t0, n):
        return [self.park_res.sub(t) for t in range(t0 // 128, (t0 + n) // 128)]

    def norm_to_h(self, bi, l, which, hT, hT_r):
        xb = [self.alloc(f"nx{i}", 128, [KC, 512]) for i in range(2)]
        sq, sq_r = self.alloc("nsq", 128, [KC, 512], BF16)
        rs, rs_r = self.alloc("nrs", 128, [512])
        tmp = [self.alloc(f"ntmp{i}", 128, [512]) for i in range(2)]
        for b, (t0, n) in enumerate(BLOCKS):
            j = 2 if b == 0 else bi
            x, x_r = xb[b % 2]
            self.dma("sp", x[:, :, 0:n], self.park[:, :, t0:t0 + n], rd=self.park_subs(t0, n), wr=[x_r])
            self.act(sq[:, :, 0:n], x[:, :, 0:n], AF.Square, rd=[x_r], wr=[sq_r])
            ps, ps_r = self.PS()
            for kc in range(KC):
                self.mm(ps[:, 0:n], self.ones_b, sq[:, kc, 0:n], start=(kc == 0), stop=(kc == KC - 1), rd=[sq_r, self.cb_r], wr=[ps_r])
            self.act(rs[:, 0:n], ps[:, 0:n], AF.Ln, rd=[ps_r, self.cc_r], wr=[rs_r], bias=self.c_eps, scale=1.0 / D)
            self.act(rs[:, 0:n], rs[:, 0:n], AF.Exp, rd=[rs_r], wr=[rs_r], scale=-0.5)
            for kc in range(KC):
                tp, tp_r = tmp[kc % 2]
                self.tt(tp[:, 0:n], x[:, kc, 0:n], rs[:, 0:n], ALU.mult, rd=[x_r, rs_r], wr=[tp_r])
                A = self.drv[:, l, j, which * 8 + kc:which * 8 + kc + 1]
                mi = 0 if which == 0 else 3
                B = self.mod[:, l, j, mi * 8 + kc:mi * 8 + kc + 1]
                self.act(hT[:, kc, t0:t0 + n], tp[:, 0:n], AF.Identity, rd=[tp_r, self.drv_r, self.mod_r], wr=[hT_r.sub(b)], bias=B, scale=A)

    def load_w(self, dst, dst_r, src2d, ncols, rows=D, cast="act", hi=False, stage=None):
        kc_n = rows // 128
        cw = ncols if ncols <= 1100 else 1024
        if stage is None:
            stage = [self.alloc(f"wstg{i}", 128, [cw], hi=hi) for i in range(2)]
        k = getattr(self, "_stg_k", 0)
        for kc in range(kc_n):
            for c0 in range(0, ncols, cw):
                c1 = min(ncols, c0 + cw)
                st, st_r = stage[k % len(stage)]
                k += 1
                self.dma("sp", st[:, 0:c1 - c0], src2d[kc * 128:(kc + 1) * 128, c0:c1], wr=[st_r])
                if cast == "act":
                    self.act(dst[:, kc, c0:c1], st[:, 0:c1 - c0], AF.Copy, rd=[st_r], wr=[dst_r.sub(kc)])
                else:
                    self.cp(dst[:, kc, c0:c1], st[:, 0:c1 - c0], rd=[st_r], wr=[dst_r.sub(kc)], eng=cast)
        self._stg_k = k
        return stage

    def layer(self, bi, l):
        d = self.dram
        m0 = self.mark()
        hT, hT_r = self.alloc("hT", 128, [KC, T], BF16)
        acc, acc_r = self.alloc("acc", 128, [KC, T], BF16)
        self.hT, self.hT_r, self.acc, self.acc_r = hT, hT_r, acc, acc_r
        m1 = self.mark()
        self.norm_to_h(bi, l, 0, hT, hT_r)
        self.release(m1)
        first = True
        for j, name in enumerate("ABCD"):
            if name not in self.stages:
                continue
            m1 = self.mark()
            yT, yT_r, lay = getattr(self, "mixer_" + name)(bi, l)
            self.merge(l, j, yT, yT_r, lay, first)
            first = False
            self.release(m1)
        if first:
            self.memset(acc[:, :, :], 0.0, wr=[acc_r])
        m1 = self.mark()
        wo, wo_r = self.alloc("w_out", 128, [KC, D], BF16)
        self.load_w(wo, wo_r, d["w_out"][l], D)
        xb = [self.alloc(f"rx{i}", 128, [KC, 512]) for i in range(2)]
        for b, (t0, n) in enumerate(BLOCKS):
            if b == 0 and l == self.NL - 1:
                continue
            j = 2 if b == 0 else bi
            x, x_r = xb[b % 2]
            self.dma("sp", x[:, :, 0:n], self.park[:, :, t0:t0 + n], rd=self.park_subs(t0, n), wr=[x_r])
            for oc in range(KC):
                ps, ps_r = self.PS()
                for kc in range(KC):
                    self.mm(ps[:, 0:n], wo[:, kc, oc * 128:(oc + 1) * 128], acc[:, kc, t0:t0 + n], start=(kc == 0), stop=(kc == KC - 1),
                            rd=[wo_r.sub(kc), acc_r], wr=[ps_r])
                g = self.mod[:, l, j, 16 + oc:16 + oc + 1]
                self.stt(x[:, oc, 0:n], ps[:, 0:n], g, x[:, oc, 0:n], ALU.mult, ALU.add, rd=[ps_r, self.mod_r, x_r], wr=[x_r])
            self.dma("sp", self.park[:, :, t0:t0 + n], x[:, :, 0:n], rd=[x_r], wr=self.park_subs(t0, n))
        self.release(m0)
        self.ffn(bi, l)

    def merge(self, l, j, yT, yT_r, lay, first):
        d = self.dram
        wg, wg_r = self.alloc("w_gate", 128, [KC, D], BF16)
        self.load_w(wg, wg_r, d["w_gate"][l, j], D)
        bst = [self.alloc(f"wbstg{i}", 128, [D]) for i in range(2)]
        if lay == 64:
            wb, wb_r = self.alloc("w_br", 64, [4, D], BF16)
            for g in range(4):
                st, st_r = bst[g % 2]
                self.dma("sp", st[0:64, :], d["w_branch"][l, j, g * 64:(g + 1) * 64, :], wr=[st_r])
                self.act(wb[:, g, :], st[0:64, :], AF.Copy, rd=[st_r], wr=[wb_r.sub(g)])
            ng = 4
        else:
            wb, wb_r = self.alloc("w_br", 128, [2, D], BF16)
            for g in range(2):
                st, st_r = bst[g % 2]
                self.dma("sp", st, d["w_branch"][l, j, g * 128:(g + 1) * 128, :], wr=[st_r])
                self.act(wb[:, g, :], st, AF.Copy, rd=[st_r], wr=[wb_r.sub(g)])
            ng = 2
        sg = [self.alloc(f"sg{i}", 128, [512]) for i in range(2)]
        tm = [self.alloc(f"mt{i}", 128, [512], BF16) for i in range(2)]
        acc, acc_r, hT, hT_r = self.acc, self.acc_r, self.hT, self.hT_r
        it = 0
        for b, (t0, n) in enumerate(BLOCKS):
            if b == 0 and l == self.NL - 1:
                continue
            for oc in range(KC):
                ps, ps_r = self.PS()
                for kc in range(KC):
                    self.mm(ps[:, 0:n], wg[:, kc, oc * 128:(oc + 1) * 128], hT[:, kc, t0:t0 + n], start=(kc == 0), stop=(kc == KC - 1),
                            rd=[wg_r.sub(kc), hT_r.sub(b)], wr=[ps_r])
                s, s_r = sg[it % 2]
                self.act(s[:, 0:n], ps[:, 0:n], AF.Sigmoid, rd=[ps_r], wr=[s_r])
                ps2, ps2_r = self.PS()
                for g in range(ng):
                    self.mm(ps2[:, 0:n], wb[:, g, oc * 128:(oc + 1) * 128], yT[:, g, t0:t0 + n], start=(g == 0), stop=(g == ng - 1),
                            rd=[wb_r.sub(g), yT_r], wr=[ps2_r])
                if first:
                    self.tt(acc[:, oc, t0:t0 + n], ps2[:, 0:n], s[:, 0:n], ALU.mult, rd=[ps2_r, s_r], wr=[acc_r.sub(b)])
                else:
                    t_, t_r = tm[it % 2]
                    self.tt(t_[:, 0:n], ps2[:, 0:n], s[:, 0:n], ALU.mult, rd=[ps2_r, s_r], wr=[t_r])
                    self.tt(acc[:, oc, t0:t0 + n], acc[:, oc, t0:t0 + n], t_[:, 0:n], ALU.add, rd=[t_r, acc_r.sub(b)], wr=[acc_r.sub(b)], eng="pool")
                it += 1

    def ffn(self, bi, l):
        d = self.dram
        last = (l == self.NL - 1)
        m0 = self.mark()
        h2, h2_r = self.alloc("h2T", 128, [KC, T], BF16, hi=True)
        m1 = self.mark()
        self.norm_to_h(bi, l, 1, h2, h2_r)
        self.release(m1)
        hid, hid_r = self.alloc("hid", 128, [FCH, T], BF16)
        m1 = self.mark()
        wa = [self.alloc(f"wa{i}", 128, [KC, 256], BF16) for i in range(2)]
        wgt = [self.alloc(f"wg{i}", 128, [KC, 256], BF16) for i in range(2)]
        sl = [self.alloc(f"fsl{i}", 128, [512]) for i in range(2)]
        it = 0
        fstg = [self.alloc(f"fstg{i}", 128, [256]) for i in range(4)]

        def ld(cg):
            a_w, a_r = wa[cg % 2]
            g_w, g_r = wgt[cg % 2]
            self.load_w(a_w, a_r, d["w_ffn_in"][l][:, cg * 256:(cg + 1) * 256], 256, cast="pool", stage=fstg)
            self.load_w(g_w, g_r, d["w_ffn_in"][l][:, FH + cg * 256:FH + (cg + 1) * 256], 256, cast="pool", stage=fstg)
        ld(0)
        for cg in range(FCH // 2):
            a_w, a_r = wa[cg % 2]
            g_w, g_r = wgt[cg % 2]
            if cg + 1 < FCH // 2:
                ld(cg + 1)
            for c2 in range(2):
                c = cg * 2 + c2
                for b, (t0, n) in enumerate(BLOCKS):
                    if b == 0 and last:
                        continue
                    pa, pa_r = self.PS()
                    for kc in range(KC):
                        self.mm(pa[:, 0:n], a_w[:, kc, c2 * 128:(c2 + 1) * 128], h2[:, kc, t0:t0 + n], start=(kc == 0), stop=(kc == KC - 1),
                                rd=[a_r.sub(kc), h2_r.sub(b)], wr=[pa_r])
                    pg, pg_r = self.PS()
                    for kc in range(KC):
                        self.mm(pg[:, 0:n], g_w[:, kc, c2 * 128:(c2 + 1) * 128], h2[:, kc, t0:t0 + n], start=(kc == 0), stop=(kc == KC - 1),
                                rd=[g_r.sub(kc), h2_r.sub(b)], wr=[pg_r])
                    s, s_r = sl[it % 2]
                    it += 1
                    self.act(s[:, 0:n], pa[:, 0:n], AF.Silu, rd=[pa_r], wr=[s_r])
                    self.tt(hid[:, c, t0:t0 + n], pg[:, 0:n], s[:, 0:n], ALU.mult, rd=[pg_r, s_r], wr=[hid_r.sub(b)])
        self.release(m1, hi=True)
        wd, wd_r = self.alloc("w_down", 128, [FCH, D], BF16)
        self.load_w(wd, wd_r, d["w_ffn_out"][l], D, rows=FH)
        xb = [self.alloc(f"fx{i}", 128, [KC, 512]) for i in range(1)]
        for b, (t0, n) in enumerate(BLOCKS):
            if b == 0 and last:
                continue
            j = 2 if b == 0 else bi
            x, x_r = xb[0]
            self.dma("sp", x[:, :, 0:n], self.park[:, :, t0:t0 + n], rd=self.park_subs(t0, n), wr=[x_r])
            for oc in range(KC):
                ps, ps_r = self.PS()
                for c in range(FCH):
                    self.mm(ps[:, 0:n], wd[:, c, oc * 128:(oc + 1) * 128], hid[:, c, t0:t0 + n], start=(c == 0), stop=(c == FCH - 1),
                            rd=[wd_r.sub(c), hid_r.sub(b)], wr=[ps_r])
                g = self.mod[:, l, j, 40 + oc:40 + oc + 1]
                self.stt(x[:, oc, 0:n], ps[:, 0:n], g, x[:, oc, 0:n], ALU.mult, ALU.add, rd=[ps_r, self.mod_r, x_r], wr=[x_r])
            self.dma("sp", self.park[:, :, t0:t0 + n], x[:, :, 0:n], rd=[x_r], wr=self.park_subs(t0, n))
        self.release(m0)

    def permute(self, to_col):
        m0 = self.mark()
        a = [self.alloc(f"pa{i}", 128, [SEQ]) for i in range(2)]
        bb = [self.alloc(f"pb{i}", 128, [SEQ]) for i in range(2)]
        lat = self.park_subs(CTXL, SEQ)
        for kc in range(KC):
            x, x_r = a[kc % 2]
            y, y_r = bb[kc % 2]
            self.dma("sp", x, self.park[:, kc, CTXL:T], rd=lat, wr=[x_r])
            if to_col:
                src = x.rearrange("p (r c) -> p c r", r=32, c=64)
                dst = y.rearrange("p (c r) -> p c r", c=64, r=32)
            else:
                src = x.rearrange("p (c r) -> p r c", c=64, r=32)
                dst = y.rearrange("p (r c) -> p r c", r=32, c=64)
            self.cp(dst, src, rd=[x_r], wr=[y_r], eng="pool")
            self.dma("sp", self.park[:, kc, CTXL:T], y, rd=[y_r], wr=lat)
        self.release(m0)

    def final(self, bi):
        if self.NL > 1:
            self.permute(to_col=False)
        m0 = self.mark()
        xb = [self.alloc(f"ox{i}", 128, [KC, 512]) for i in range(2)]
        sq, sq_r = self.alloc("osq", 128, [KC, 512], BF16)
        rs, rs_r = self.alloc("ors", 128, [512])
        xn, xn_r = self.alloc("oxn", 128, [KC, 512])
        ot = [self.alloc(f"ot{i}", 128, [D]) for i in range(2)]
        fn = self.P("fnT")
        it = 0
        for b, (t0, n) in enumerate(BLOCKS):
            if b == 0:
                continue
            x, x_r = xb[b % 2]
            self.dma("sp", x[:, :, 0:n], self.park[:, :, t0:t0 + n], rd=self.park_subs(t0, n), wr=[x_r])
            self.act(sq[:, :, 0:n], x[:, :, 0:n], AF.Square, rd=[x_r], wr=[sq_r])
            ps, ps_r = self.PS()
            for kc in range(KC):
                self.mm(ps[:, 0:n], self.ones_b, sq[:, kc, 0:n], start=(kc == 0), stop=(kc == KC - 1), rd=[sq_r, self.cb_r], wr=[ps_r])
            self.act(rs[:, 0:n], ps[:, 0:n], AF.Ln, rd=[ps_r, self.cc_r], wr=[rs_r], bias=self.c_eps, scale=1.0 / D)
            self.act(rs[:, 0:n], rs[:, 0:n], AF.Exp, rd=[rs_r], wr=[rs_r], scale=-0.5)
            for kc in range(KC):
                self.stt(xn[:, kc, 0:n], x[:, kc, 0:n], fn[:, kc:kc + 1], rs[:, 0:n], ALU.mult, ALU.mult, rd=[x_r, rs_r, self.prm_r], wr=[xn_r])
            for tl in range(n // 128):
                o, o_r = ot[it % 2]
                it += 1
                for half in range(2):
                    ps, ps_r = self.PS()
                    for q in range(4):
                        kc = half * 4 + q
                        self.tr(ps[:, q * 128:(q + 1) * 128], xn[:, kc, tl * 128:(tl + 1) * 128], self.ident_f, rd=[xn_r, self.cf_r], wr=[ps_r])
                    self.act(o[:, half * 512:(half + 1) * 512], ps[:, :], AF.Copy, rd=[ps_r], wr=[o_r])
                tok = t0 - CTXL + tl * 128
                self.dma("sp", self.yout[bi, tok:tok + 128, :], o, rd=[o_r], wr=[self.yres])
        self.release(m0)

    def inproj(self, w, w_r, col, M, b, t0, n):
        ps, ps_r = self.PS()
        for kc in range(KC):
            self.mm(ps[0:M, 0:n], w[:, kc, col:col + M], self.hT[:, kc, t0:t0 + n], start=(kc == 0), stop=(kc == KC - 1),
                    rd=[w_r.sub(kc), self.hT_r.sub(b)], wr=[ps_r])
        return ps, ps_r

    def post_norm_gate(self, l, oT, oT_r, nw, w, w_r, gcol, gbias, func, yT, yT_r):
        sq, sq_r = self.alloc("pn_sq", 64, [4, 512], BF16)
        rs, rs_r = self.alloc("pn_rs", 64, [512])
        gs, gs_r = self.alloc("pn_gs", 64, [512])
        tp, tp_r = self.alloc("pn_tp", 64, [512])
        for b, (t0, n) in enumerate(BLOCKS):
            if b == 0 and l == self.NL - 1:
                continue
            self.act(sq[:, :, 0:n], oT[:, :, t0:t0 + n], AF.Square, rd=[oT_r], wr=[sq_r])
            for h in range(4):
                ps, ps_r = self.PS()
                self.mm(ps[0:64, 0:n], self.ones_b[0:64, 0:64], sq[:, h, 0:n], rd=[sq_r, self.cb_r], wr=[ps_r])
                self.act(rs[:, 0:n], ps[0:64, 0:n], AF.Ln, rd=[ps_r, self.cc_r], wr=[rs_r], bias=self.c_eps[0:64, :], scale=1.0 / 64)
                self.act(rs[:, 0:n], rs[:, 0:n], AF.Exp, rd=[rs_r], wr=[rs_r], scale=-0.5)
                pg, pg_r = self.inproj(w, w_r, gcol + h * 64, 64, b, t0, n)
                self.act(gs[:, 0:n], pg[0:64, 0:n], func, rd=[pg_r, self.prm_r], wr=[gs_r], bias=gbias[:, h:h + 1])
                self.tt(tp[:, 0:n], oT[:, h, t0:t0 + n], rs[:, 0:n], ALU.mult, rd=[oT_r, rs_r], wr=[tp_r])
                self.stt(yT[:, h, t0:t0 + n], tp[:, 0:n], nw[:, h:h + 1], gs[:, 0:n], ALU.mult, ALU.mult, rd=[tp_r, gs_r, self.prm_r], wr=[yT_r])

    def mixer_C(self, bi, l):
        d = self.dram
        last = (l == self.NL - 1)
        yT, yT_r = self.alloc("yC", 64, [4, T], BF16)
        ms = self.mark()
        w, w_r = self.alloc("wC", 128, [KC, 512], BF16)
        self.load_w(w, w_r, d["w_in"][l][:, 1808:2320], 512)
        gw, gw_r = self.alloc("gw", 128, [4, 128], BF16)
        self.dma("pool", gw, d["gwsT"][:, l], wr=[gw_r])
        gb, gb_r = self.alloc("gb", 1, [512], BF16)
        self.dma("pool", gb, d["gbs"][0:1, l * 512:(l + 1) * 512], wr=[gb_r])
        bC = self.P("bC").rearrange("p (l g) -> p l g", l=2, g=8)
        gnw = self.P("gnw").rearrange("p (l g) -> p l g", l=2, g=4)
        gnb = self.P("gnb").rearrange("p (l g) -> p l g", l=2, g=4)
        vf, vf_r = self.alloc("vf", 64, [4, T])
        vn, vn_r = self.alloc("vn", 64, [4, T], BF16)
        vbs = [self.alloc(f"vb{i}", 64, [4, 512], BF16) for i in range(2)]
        vqs = [self.alloc(f"vq{i}", 64, [4, 512], BF16) for i in range(2)]
        mus = [self.alloc(f"mu{i}", 64, [512]) for i in range(2)]
        vas = [self.alloc(f"va{i}", 64, [512]) for i in range(2)]
        tqs = [self.alloc(f"tq{i}", 64, [512]) for i in range(4)]
        vts = [self.alloc(f"vt{i}", 128, [4, 64], BF16) for i in range(3)]
        blocks = [(b, t0, n) for b, (t0, n) in enumerate(BLOCKS) if not (b == 0 and last)]
        for (b, t0, n) in blocks:
            for g in range(8):
                ps, ps_r = self.inproj(w, w_r, g * 64, 64, b, t0, n)
                if g < 4:
                    self.act(yT[:, g, t0:t0 + n], ps[0:64, 0:n], AF.Gelu_apprx_tanh, rd=[ps_r, self.prm_r], wr=[yT_r.sub(b)], bias=bC[:, l, g:g + 1])
                else:
                    self.act(vf[:, g - 4, t0:t0 + n], ps[0:64, 0:n], AF.Gelu_apprx_tanh, rd=[ps_r, self.prm_r], wr=[vf_r.sub(b)], bias=bC[:, l, g:g + 1])
        for i, (b, t0, n) in enumerate(blocks):
            vb, vb_r = vbs[i % 2]
            vq, vq_r = vqs[i % 2]
            mu, mu_r = mus[i % 2]
            va, va_r = vas[i % 2]
            self.cp(vb[:, :, 0:n], vf[:, :, t0:t0 + n], rd=[vf_r.sub(b)], wr=[vb_r], eng="pool")
            self.act(vq[:, :, 0:n], vf[:, :, t0:t0 + n], AF.Square, rd=[vf_r.sub(b)], wr=[vq_r])
            pm, pm_r = self.PS()
            for g in range(4):
                self.mm(pm[0:64, 0:n], self.ones_b[0:64, 0:64], vb[:, g, 0:n], start=(g == 0), stop=(g == 3), rd=[vb_r, self.cb_r], wr=[pm_r])
            pq, pq_r = self.PS()
            for g in range(4):
                self.mm(pq[0:64, 0:n], self.ones_b[0:64, 0:64], vq[:, g, 0:n], start=(g == 0), stop=(g == 3), rd=[vq_r, self.cb_r], wr=[pq_r])
            tq, tq_r = tqs[0]
            self.act(mu[:, 0:n], pm[0:64, 0:n], AF.Copy, rd=[pm_r], wr=[mu_r], scale=1.0 / 256)
            self.tt(tq[:, 0:n], mu[:, 0:n], mu[:, 0:n], ALU.mult, rd=[mu_r], wr=[tq_r])
            self.stt(va[:, 0:n], pq[0:64, 0:n], 1.0 / 256, tq[:, 0:n], ALU.mult, ALU.subtract, rd=[pq_r, tq_r], wr=[va_r])
            self.act(va[:, 0:n], va[:, 0:n], AF.Ln, rd=[va_r, self.cc_r], wr=[va_r], bias=self.c_eps[0:64, :])
            self.act(va[:, 0:n], va[:, 0:n], AF.Exp, rd=[va_r], wr=[va_r], scale=-0.5)
            for g in range(4):
                tq, tq_r = tqs[g]
                self.tt(tq[:, 0:n], vf[:, g, t0:t0 + n], mu[:, 0:n], ALU.subtract, rd=[vf_r.sub(b), mu_r], wr=[tq_r])
                self.tt(tq[:, 0:n], tq[:, 0:n], va[:, 0:n], ALU.mult, rd=[tq_r, va_r], wr=[tq_r])
            for g in range(4):
                tq, tq_r = tqs[g]
                self.act(vn[:, g, t0:t0 + n], tq[:, 0:n], AF.Identity, rd=[tq_r, self.prm_r], wr=[vn_r.sub(b)], bias=gnb[:, l, g:g + 1], scale=gnw[:, l, g:g + 1])
        it = 0
        for (b, t0, n) in blocks:
            for tl in range(n // 128):
                tk = t0 + tl * 128
                pb, pb_r = self.PSB()
                for g in range(4):
                    self.tr(pb[:, g * 64:(g + 1) * 64], vn[:, g, tk:tk + 128], self.ident_b[0:64, 0:64], rd=[vn_r.sub(b), self.cb_r], wr=[pb_r])
                vt, vt_r = vts[it % 3]
                it += 1
                self.act(vt, pb[:, 0:256].rearrange("p (g c) -> p g c", g=4, c=64), AF.Copy, rd=[pb_r], wr=[vt_r])
                ps, ps_r = self.PS()
                for g in range(4):
                    gs = slice(g * 128, (g + 1) * 128)
                    self.mm(ps[0:64, gs], vt[:, g, :], gw[:, g, :], start=True, stop=False, rd=[vt_r, gw_r], wr=[ps_r])
                    self.mm(ps[0:64, gs], self.ones_b[0:1, 0:64], gb[0:1, g * 128:(g + 1) * 128], start=False, stop=True, rd=[gb_r, self.cb_r], wr=[ps_r])
                self.tt(yT[:, :, tk:tk + 128], ps[0:64, 0:512].rearrange("p (g t) -> p g t", g=4, t=128), yT[:, :, tk:tk + 128], ALU.mult,
                        rd=[ps_r, yT_r.sub(b)], wr=[yT_r.sub(b)])
        self.release(ms)
        return yT, yT_r, 64

    def mixer_A(self, bi, l):
        d = self.dram
        last = (l == self.NL - 1)
        ha, ha_r = self.alloc("ha", 64, [4, T], BF16)
        yT, yT_r = ha, ha_r
        ms = self.mark()
        m2 = self.mark()
        w, w_r = self.alloc("wA", 128, [KC, 1040], BF16, hi=True)
        self.load_w(w, w_r, d["w_in"][l][:, 0:1040], 1040, hi=True)
        bA = self.P("bA").rearrange("p (l g) -> p l g", l=2, g=16)
        bG = self.P("bGrow").rearrange("p (l g) -> p l g", l=2, g=16)
        mnw = self.P("mnw").rearrange("p (l g) -> p l g", l=2, g=4)
        qk, qk_r = self.alloc("qk", 64, [8, T], BF16)
        ktok, ktok_r = self.alloc("ktok", 128, [NT, 256], BF16)
        vones, vones_r = self.alloc("vtok", 128, [NT, 4, 64], BF16)
        gtok, gtok_r = self.alloc("gtok", 128, [NT, 16], hi=True)
        alpha, alpha_r = self.alloc("alpha", 128, [NT, 8])
        rows, rows_r = self.alloc("rows", 4, [2, T])
        vtmp, vtmp_r = self.alloc("vtmp", 64, [4, 512], BF16, hi=True)
        for b, (t0, n) in enumerate(BLOCKS):
            for g in range(12):
                ps, ps_r = self.inproj(w, w_r, g * 64, 64, b, t0, n)
                if g < 8:
                    self.act(qk[:, g, t0:t0 + n], ps[0:64, 0:n], AF.Identity, rd=[ps_r, self.prm_r], wr=[qk_r], bias=bA[:, l, g:g + 1])
                else:
                    self.act(vtmp[:, g - 8, 0:n], ps[0:64, 0:n], AF.Identity, rd=[ps_r, self.prm_r], wr=[vtmp_r], bias=bA[:, l, g:g + 1])
            for tl in range(n // 128):
                t = t0 // 128 + tl
                pb, pb_r = self.PSB()
                for h in range(4):
                    self.tr(pb[:, h * 64:(h + 1) * 64], qk[:, 4 + h, t * 128:(t + 1) * 128], self.ident_b[0:64, 0:64], rd=[qk_r, self.cb_r], wr=[pb_r])
                    self.tr(pb[:, 256 + h * 64:256 + (h + 1) * 64], vtmp[:, h, tl * 128:(tl + 1) * 128], self.ident_b[0:64, 0:64], rd=[vtmp_r, self.cb_r], wr=[pb_r])
                self.cp(ktok[:, t, :], pb[:, 0:256], rd=[pb_r], wr=[ktok_r])
                self.cp(vones[:, t, :, :], pb[:, 256:512].rearrange("p (h c) -> p h c", h=4, c=64), rd=[pb_r], wr=[vones_r])
                pg, pg_r = self.PS()
                for kc in range(KC):
                    self.mm(pg[:, 0:16], self.hT[:, kc, t * 128:(t + 1) * 128], w[:, kc, 1024:1040], start=(kc == 0), stop=(kc == KC - 1),
                            rd=[w_r.sub(kc), self.hT_r.sub(b)], wr=[pg_r])
                self.tt(gtok[:, t, :], pg[:, 0:16], bG[:, l, :], ALU.add, rd=[pg_r, self.prm_r], wr=[gtok_r])
        dcall, dcall_r = self.alloc("dcall", 64, [NT, 8])
        cstk = [self.alloc(f"cstk{i}", 128, [8]) for i in range(2)]
        g4 = gtok.rearrange("p t (a c) -> p t a c", a=4, c=4)
        for a in (1, 3):
            self.act(g4[:, :, a, :], g4[:, :, a, :], AF.Exp, rd=[gtok_r], wr=[gtok_r], scale=-1.0)
            self.act(g4[:, :, a, :], g4[:, :, a, :], AF.Ln, rd=[gtok_r, self.cc_r], wr=[gtok_r], bias=self.c_one)
        for t in range(NT):
            pc, pc_r = self.PS()
            self.mm(pc[:, 0:4], self.triF, gtok[:, t, 4:8], rd=[gtok_r, self.cf_r], wr=[pc_r])
            self.mm(pc[:, 4:8], self.triB, gtok[:, t, 12:16], rd=[gtok_r, self.cf_r], wr=[pc_r])
            self.mm(pc[0:4, 128:256], gtok[:, t, 4:8], self.triF, rd=[gtok_r, self.cf_r], wr=[pc_r])
            self.mm(pc[0:4, 256:384], gtok[:, t, 12:16], self.triB, rd=[gtok_r, self.cf_r], wr=[pc_r])
            self.tt(alpha[:, t, 0:4], pc[:, 0:4], gtok[:, t, 0:4], ALU.add, rd=[pc_r, gtok_r], wr=[alpha_r])
            self.tt(alpha[:, t, 4:8], pc[:, 4:8], gtok[:, t, 8:12], ALU.add, rd=[pc_r, gtok_r], wr=[alpha_r])
            self.cp(rows[:, 0, t * 128:(t + 1) * 128], pc[0:4, 128:256], rd=[pc_r], wr=[rows_r])
            self.cp(rows[:, 1, t * 128:(t + 1) * 128], pc[0:4, 256:384], rd=[pc_r], wr=[rows_r])
            ck, ck_r = cstk[t % 2]
            self.act(ck, pc[:, 0:8], AF.Copy, rd=[pc_r], wr=[ck_r])
            pe2, pe2_r = self.PS()
            self.mm(pe2[0:64, 0:4], self.pick_last, ck[:, 0:4], rd=[ck_r, self.cf_r], wr=[pe2_r])
            self.mm(pe2[0:64, 4:8], self.pick_first, ck[:, 4:8], rd=[ck_r, self.cf_r], wr=[pe2_r])
            self.act(dcall[:, t, :], pe2[0:64, 0:8], AF.Exp, rd=[pe2_r], wr=[dcall_r], scale=-1.0)
        self.act(alpha[:, :, :], alpha[:, :, :], AF.Exp, rd=[alpha_r, self.cc_r], wr=[alpha_r], bias=self.c_nl8)
        self.R.barrier()
        self.hi = self.AW
        Cst = [self.alloc(f"Cst{i}", 64, [4, 128]) for i in range(2)]
        Cbf = [[self.alloc(f"Cbf{i}{p}", 64, [4, 128], BF16) for p in range(2)] for i in range(2)]
        for i in range(2):
            self.memset(Cst[i][0][:, :, :], 0.0, wr=[Cst[i][1]])
            self.memset(Cbf[i][0][0][:, :, :], 0.0, wr=[Cbf[i][0][1]])
        vps = [self.alloc(f"vp{i}", 128, [4, 128], BF16) for i in range(3)]
        WTs = [self.alloc(f"WT{i}", 128, [4, 128], BF16) for i in range(3)]
        Ens = [self.alloc(f"En{i}", 64, [4, 128]) for i in range(2)]
        aYs = [self.alloc(f"aY{i}", 64, [4, 128]) for i in range(2)]
        hts = [self.alloc(f"hb{i}", 64, [4, 128], BF16) for i in range(2)]
        tcs = [self.alloc(f"tc{i}", 64, [4, 128]) for i in range(2)]
        orders = [list(range(NT)), [1, 0] + list(range(NT - 1, 1, -1))]
        written = set()
        it = 0
        LB = [(self.psf[i], self.psf_res[i]) for i in range(4)]
        SB = [(self.psf[i], self.psf_res[i]) for i in (4, 5)]
        li = [0]
        si = [0]

        def nextL():
            li[0] += 1
            return LB[li[0] % 4]

        def nextS():
            si[0] += 1
            return SB[si[0] % 2]

        def v4(ap):
            return ap.rearrange("p (h t) -> p h t", h=4, t=128)
        pending = None

        def tail(args):
            t, px, px_r, aY, aY_r, En, En_r, k = args
            tsl = slice(t * 128, (t + 1) * 128)
            self.tt(aY, aY, En, ALU.max, rd=[aY_r, En_r], wr=[aY_r])
            self.recip(aY, aY, rd=[aY_r], wr=[aY_r])
            hview = ha[:, :, tsl]
            if t not in written:
                written.add(t)
                self.tt(hview, v4(px[0:64, 0:512]), aY, ALU.mult, rd=[px_r, aY_r], wr=[ha_r.sub(t)])
            else:
                hb, hb_r = hts[k % 2]
                self.tt(hb, v4(px[0:64, 0:512]), aY, ALU.mult, rd=[px_r, aY_r], wr=[hb_r])
                self.tt(hview, hview, hb, ALU.add, rd=[hb_r, ha_r.sub(t)], wr=[ha_r.sub(t)], eng="pool")
        for sidx in range(NT):
            for dd in range(2):
                t = orders[dd][sidx]
                it += 1
                mask = self.maskF if dd == 0 else self.maskB
                skip_out = last and t < 2
                tsl = slice(t * 128, (t + 1) * 128)
                a_b = alpha[:, t, dd * 4:dd * 4 + 4].unsqueeze(2).to_broadcast([128, 4, 64])
                cs_, cs_r_ = Cst[dd]
                cb_cur, cb_cur_r = Cbf[dd][sidx % 2]
                cb_nxt, cb_nxt_r = Cbf[dd][(sidx + 1) % 2]
                vp, vp_r = vps[it % 3]
                self.tt(vp[:, :, 0:64], vones[:, t, :, :], a_b, ALU.mult, rd=[vones_r, alpha_r], wr=[vp_r], eng="pool")
                self.cp(vp[:, :, 64:128], a_b, rd=[alpha_r], wr=[vp_r], eng="pool")
                pu, pu_r = nextS()
                for h in range(4):
                    self.mm(pu[0:64, h * 128:(h + 1) * 128], ktok[:, t, h * 64:(h + 1) * 64], vp[:, h, :], rd=[ktok_r, vp_r], wr=[pu_r])
                tc, tc_r = tcs[it % 2]
                self.tt(tc, v4(pu[0:64, 0:512]), cs_, ALU.add, rd=[pu_r, cs_r_], wr=[tc_r])
                self.tt(cs_, tc, dcall[:, t, dd * 4:dd * 4 + 4].unsqueeze(2).to_broadcast([64, 4, 128]), ALU.mult,
                        rd=[tc_r, dcall_r], wr=[cs_r_])
                self.act(cb_nxt, cs_, AF.Copy, rd=[cs_r_], wr=[cb_nxt_r])
                cur = None
                if not skip_out:
                    pB, pB_r = nextS()
                    for h in range(4):
                        self.mm(pB[0:64, h * 128:(h + 1) * 128], self.selrow[:, h, :], rows[:, dd, tsl], rd=[rows_r, self.cf_r], wr=[pB_r])
                    En, En_r = Ens[it % 2]
                    self.act(En, v4(pB[0:64, 0:512]), AF.Exp, rd=[pB_r], wr=[En_r])
                    pst, pst_r = nextS()
                    for h in range(4):
                        self.mm(pst[:, h * 128:(h + 1) * 128], qk[:, 4 + h, tsl], qk[:, h, tsl], rd=[qk_r], wr=[pst_r])
                    WT, WT_r = WTs[it % 3]
                    self.tt(WT, v4(pst[:, 0:512]), mask.unsqueeze(1).to_broadcast([128, 4, 128]), ALU.mult, rd=[pst_r, self.cb_r], wr=[WT_r])
                    px, px_r = nextL()
                    for h in range(4):
                        hs = slice(h * 128, (h + 1) * 128)
                        self.mm(px[0:64, hs], vp[:, h, 0:64], WT[:, h, :], start=True, stop=False, rd=[vp_r, WT_r], wr=[px_r])
                        self.mm(px[0:64, hs], cb_cur[:, h, 0:64], qk[:, h, tsl], start=False, stop=True, rd=[cb_cur_r, qk_r], wr=[px_r])
                    py, py_r = nextL()
                    for h in range(4):
                        hs = slice(h * 128, (h + 1) * 128)
                        self.mm(py[0:64, hs], vp[:, h, 64:128], WT[:, h, :], start=True, stop=False, rd=[vp_r, WT_r], wr=[py_r])
                        self.mm(py[0:64, hs], cb_cur[:, h, 64:128], qk[:, h, tsl], start=False, stop=True, rd=[cb_cur_r, qk_r], wr=[py_r])
                    aY, aY_r = aYs[it % 2]
                    self.act(aY, v4(py[0:64, 0:512]), AF.Abs, rd=[py_r], wr=[aY_r])
                    cur = (t, px, px_r, aY, aY_r, En, En_r, it)
                if pending is not None:
                    tail(pending)
                pending = cur
        if pending is not None:
            tail(pending)
        self.dbg_dump(f"dbg_ha{l}", ha[:, :, CTXL:T], ha_r, [64, 4, SEQ], BF16)
        self.release(m2)
        wo, wo_r = self.alloc("wAo", 128, [KC, 256], BF16)
        self.load_w(wo, wo_r, d["w_in"][l][:, 768:1024], 256)
        self.post_norm_gate(l, ha, ha_r, mnw[:, l, :], wo, wo_r, 0, bA[:, l, 12:16], AF.Sigmoid, yT, yT_r)
        self.release(ms)
        return yT, yT_r, 64

    def mixer_D(self, bi, l):
        d = self.dram
        last = (l == self.NL - 1)
        oT, oT_r = self.alloc("oD", 64, [4, T], BF16)
        ms = self.mark()
        bD = self.P("bD").rearrange("p (l g) -> p l g", l=2, g=20)
        hnw = self.P("hnw").rearrange("p (l g) -> p l g", l=2, g=4)
        lbc, oml = self.lb[:, l, 0, :], self.lb[:, l, 1, :]
        qT, qT_r = self.alloc("qD", 64, [4, T], BF16)
        kT, kT_r = self.alloc("kD", 64, [4, T], BF16)
        cs, cs_r = self.alloc("csD", 64, [4, T])
        vtok, vtok_r = self.alloc("vtokD", 128, [NT, 4, 64], BF16)
        lx, lx_r = self.alloc("lxD", 64, [8])
        self.ts(lx[:, 0:4], lbc, 1e-30, ALU.add, rd=[self.lb_r], wr=[lx_r])
        self.ts(lx[:, 4:8], oml, -1.0, ALU.mult, rd=[self.lb_r], wr=[lx_r])
        m3 = self.mark()
        w, w_r = self.alloc("wDq", 128, [KC, 512], BF16, hi=True)
        self.load_w(w, w_r, d["w_in"][l][:, 2320:2832], 512, hi=True)
        vtmp, vtmp_r = self.alloc("vtmpD", 64, [4, 512], BF16)
        for b, (t0, n) in enumerate(BLOCKS):
            for h in range(4):
                ps, ps_r = self.inproj(w, w_r, h * 64, 64, b, t0, n)
                self.act(qT[:, h, t0:t0 + n], ps[0:64, 0:n], AF.Silu, rd=[ps_r, self.prm_r], wr=[qT_r], bias=bD[:, l, h:h + 1])
                ps, ps_r = self.inproj(w, w_r, (4 + h) * 64, 64, b, t0, n)
                self.act(vtmp[:, h, 0:n], ps[0:64, 0:n], AF.Identity, rd=[ps_r, self.prm_r], wr=[vtmp_r], bias=bD[:, l, 4 + h:5 + h])
            for tl in range(n // 128):
                t = t0 // 128 + tl
                pb, pb_r = self.PSB()
                for h in range(4):
                    self.tr(pb[:, h * 64:(h + 1) * 64], vtmp[:, h, tl * 128:(tl + 1) * 128], self.ident_b[0:64, 0:64], rd=[vtmp_r, self.cb_r], wr=[pb_r])
                self.cp(vtok[:, t, :, :], pb[:, 0:256].rearrange("p (h c) -> p h c", h=4, c=64), rd=[pb_r], wr=[vtok_r])
        self.release(m3, hi=True)
        for dd in range(2):
            m3 = self.mark()
            w, w_r = self.alloc("wDf", 128, [KC, 256], BF16, hi=True)
            self.load_w(w, w_r, d["w_in"][l][:, 2832 + dd * 256:3088 + dd * 256], 256, hi=True)
            rm, rm_r = self.alloc("rmD", 64, [T], BF16, hi=True)
            self.dma("sp", rm, d["rmask"][:, dd, :], wr=[rm_r])
            sbs = [self.alloc(f"sgD{i}", 64, [512]) for i in range(4)]
            for b, (t0, n) in enumerate(BLOCKS):
                for h in range(4):
                    ps, ps_r = self.inproj(w, w_r, h * 64, 64, b, t0, n)
                    sb_, sb_r = sbs[h]
                    self.act(sb_[:, 0:n], ps[0:64, 0:n], AF.Sigmoid, rd=[ps_r, self.prm_r], wr=[sb_r],
                             bias=bD[:, l, 8 + dd * 4 + h:8 + dd * 4 + h + 1])
                for h in range(4):
                    sb_, sb_r = sbs[h]
                    self.act(cs[:, h, t0:t0 + n], sb_[:, 0:n], AF.Ln, rd=[sb_r, lx_r], wr=[cs_r], scale=oml[:, h:h + 1], bias=lx[:, h:h + 1])
                    self.ts(kT[:, h, t0:t0 + n], sb_[:, 0:n], lx[:, 4 + h:5 + h], ALU.mult, s2=oml[:, h:h + 1], op1=ALU.add,
                            rd=[sb_r, lx_r, self.lb_r], wr=[kT_r])
            for h in range(4):
                if dd == 0:
                    self.scan(cs[:, h, :], rm[:, :], cs[:, h, :], 0.0, ALU.mult, ALU.subtract, rd=[cs_r, rm_r], wr=[cs_r])
                else:
                    self.scan(cs[:, h, ::-1], rm[:, ::-1], cs[:, h, ::-1], 0.0, ALU.mult, ALU.subtract, rd=[cs_r, rm_r], wr=[cs_r])
            self.release(m3, hi=True)
            m3 = self.mark()
            Sst, Sst_r = self.alloc("Sst", 64, [4, 64])
            Sbfs = [self.alloc(f"Sbf{i}", 64, [4, 64], BF16) for i in range(2)]
            self.memset(Sst[:, :, :], 0.0, wr=[Sst_r])
            self.memset(Sbfs[0][0][:, :, :], 0.0, wr=[Sbfs[0][1]])
            csref, csref_r = self.alloc("csref", 64, [4, 4])
            decs = [self.alloc(f"decD{i}", 64, [4]) for i in range(2)]
            EA = [self.alloc(f"EA{i}", 64, [4, 128]) for i in range(2)]
            EK = [self.alloc(f"EK{j}", 64, [4, 32 * (j + 1) if dd == 0 else 128 - 32 * j]) for j in range(4)]
            Ql, Ql_r = self.alloc("Ql", 64, [4, 128], BF16)
            Qgs = [self.alloc(f"Qg{i}", 64, [4, 128], BF16) for i in range(2)]
            Ke, Ke_r = self.alloc("Ke", 64, [4, 128], BF16)
            Kj = [self.alloc(f"Kj{i}", 64, [4, 128], BF16) for i in range(4)]
            for i in range(4):
                self.memset(Kj[i][0][:, :, :], 0.0, wr=[Kj[i][1]])
            ktb, ktb_r = self.alloc("ktb", 128, [256], BF16)
            WTs = [self.alloc(f"WTd{i}", 128, [4, 128], BF16) for i in range(2)]
            Stmp, Stmp_r = self.alloc("Stmp", 64, [4, 64])
            order = list(range(NT)) if dd == 0 else [1, 0] + list(range(NT - 1, 1, -1))
            mask = self.maskF if dd == 0 else self.maskB
            LBk = [(self.psf[i], self.psf_res[i]) for i in (0, 1)]
            SBk = [(self.psf[i], self.psf_res[i]) for i in (2, 3, 4, 5)]
            sbi = [0]

            def nextS():
                sbi[0] += 1
                return SBk[sbi[0] % 4]
            kr = [((0, 32 * (j + 1)) if dd == 0 else (32 * j, 128)) for j in range(4)]

            def prep(t, slot):
                skip_out = last and t < 2
                b0 = t * 128
                tsl = slice(b0, b0 + 128)
                cst = cs[:, :, tsl]
                dec, dec_r = decs[slot]
                Qg, Qg_r = Qgs[slot]
                self.memset(csref[:, :, :], 0.0, wr=[csref_r])
                if dd == 0:
                    self.cp(csref[:, :, 1:4], cs[:, :, b0 + 31:b0 + 96:32], rd=[cs_r], wr=[csref_r])
                    endc = cs[:, :, b0 + 127:b0 + 128]
                else:
                    self.cp(csref[:, :, 0:3], cs[:, :, b0 + 32:b0 + 97:32], rd=[cs_r], wr=[csref_r])
                    endc = cs[:, :, b0:b0 + 1]
                self.act(dec.rearrange("p (h o) -> p h o", h=4, o=1), endc, AF.Exp, rd=[cs_r], wr=[dec_r], scale=-1.0)
                (Eq, Eq_r), (Ee, Ee_r) = EA[0], EA[1]
                if not skip_out:
                    self.tt(Eq.rearrange("p h (j c) -> p h j c", j=4, c=32), cst.rearrange("p h (j c) -> p h j c", j=4, c=32),
                            csref.unsqueeze(3).to_broadcast([64, 4, 4, 32]), ALU.subtract, rd=[cs_r, csref_r], wr=[Eq_r])
                    for j in range(4):
                        k0, k1 = kr[j]
                        self.tt(EK[j][0], cst[:, :, k0:k1], csref[:, :, j:j + 1].to_broadcast([64, 4, k1 - k0]), ALU.subtract,
                                rd=[cs_r, csref_r], wr=[EK[j][1]], eng=("pool" if j % 2 == 0 else "dve"))
                self.tt(Ee, cst, endc.to_broadcast([64, 4, 128]), ALU.subtract, rd=[cs_r], wr=[Ee_r], eng="pool")
                if not skip_out:
                    self.act(Ql, Eq, AF.Exp, rd=[Eq_r], wr=[Ql_r], scale=-1.0)
                    self.act(Qg, cst, AF.Exp, rd=[cs_r], wr=[Qg_r], scale=-1.0)
                    for j in range(4):
                        k0, k1 = kr[j]
                        self.act(Kj[j][0][:, :, k0:k1], EK[j][0], AF.Exp, rd=[EK[j][1]], wr=[Kj[j][1]])
                self.act(Ke, Ee, AF.Exp, rd=[Ee_r], wr=[Ke_r])
                if not skip_out:
                    self.tt(Ql, Ql, qT[:, :, tsl], ALU.mult, rd=[Ql_r, qT_r], wr=[Ql_r])
                    self.tt(Qg, Qg, qT[:, :, tsl], ALU.mult, rd=[Qg_r, qT_r], wr=[Qg_r])
                    for j in range(4):
                        k0, k1 = kr[j]
                        self.stt(Kj[j][0][:, :, k0:k1], Kj[j][0][:, :, k0:k1], 1.0e26, kT[:, :, b0 + k0:b0 + k1], ALU.min, ALU.mult,
                                 rd=[Kj[j][1], kT_r], wr=[Kj[j][1]])
                self.tt(Ke, Ke, kT[:, :, tsl], ALU.mult, rd=[Ke_r, kT_r], wr=[Ke_r])
                pb, pb_r = self.PSB()
                for h in range(4):
                    self.tr(pb[:, h * 64:(h + 1) * 64], Ke[:, h, :], self.ident_b[0:64, 0:64], rd=[Ke_r, self.cb_r], wr=[pb_r])
                self.act(ktb, pb[:, 0:256], AF.Copy, rd=[pb_r], wr=[ktb_r])
                ps3, ps3_r = LBk[slot]
                for h in range(4):
                    self.mm(ps3[0:64, h * 64:(h + 1) * 64], ktb[:, h * 64:(h + 1) * 64], vtok[:, t, h, :], rd=[ktb_r, vtok_r], wr=[ps3_r])
                if not skip_out:
                    ps, ps_r = nextS()
                    for h in range(4):
                        for j in range(4):
                            self.mm(ps[:, h * 128 + j * 32:h * 128 + (j + 1) * 32], Kj[j][0][:, h, :], Ql[:, h, j * 32:(j + 1) * 32],
                                    rd=[Kj[j][1], Ql_r], wr=[ps_r])
                    WT, WT_r = WTs[slot]
                    self.tt(WT, ps[:, 0:512].rearrange("p (h t) -> p h t", h=4, t=128), mask.unsqueeze(1).to_broadcast([128, 4, 128]),
                            ALU.mult, rd=[ps_r, self.cb_r], wr=[WT_r])

            def chain(t, slot, k):
                skip_out = last and t < 2
                tsl = slice(t * 128, (t + 1) * 128)
                dec, dec_r = decs[slot]
                Qg, Qg_r = Qgs[slot]
                ps3, ps3_r = LBk[slot]
                cur, cur_r = Sbfs[k % 2]
                nxt, nxt_r = Sbfs[(k + 1) % 2]
                self.tt(Stmp, Sst, dec.unsqueeze(2).to_broadcast([64, 4, 64]), ALU.mult, rd=[Sst_r, dec_r], wr=[Stmp_r])
                self.tt(Sst, Stmp, ps3[0:64, 0:256].rearrange("p (h c) -> p h c", h=4, c=64), ALU.add, rd=[ps3_r, Stmp_r], wr=[Sst_r])
                self.act(nxt, Sst, AF.Copy, rd=[Sst_r], wr=[nxt_r])
                if not skip_out:
                    WT, WT_r = WTs[slot]
                    ps2, ps2_r = nextS()
                    for h in range(4):
                        hs = slice(h * 128, (h + 1) * 128)
                        self.mm(ps2[0:64, hs], vtok[:, t, h, :], WT[:, h, :], start=True, stop=False, rd=[vtok_r, WT_r], wr=[ps2_r])
                        self.mm(ps2[0:64, hs], cur[:, h, :], Qg[:, h, :], start=False, stop=True, rd=[cur_r, Qg_r], wr=[ps2_r])
                    p2v = ps2[0:64, 0:512].rearrange("p (h t) -> p h t", h=4, t=128)
                    if dd == 0:
                        self.act(oT[:, :, tsl], p2v, AF.Copy, rd=[ps2_r], wr=[oT_r.sub(t)])
                    else:
                        self.tt(oT[:, :, tsl], p2v, oT[:, :, tsl], ALU.add, rd=[ps2_r, oT_r.sub(t)], wr=[oT_r.sub(t)])
            prep(order[0], 0)
            for i, t in enumerate(order):
                if i + 1 < NT:
                    prep(order[i + 1], (i + 1) % 2)
                chain(t, i % 2, i)
            self.release(m3)
        self.dbg_dump(f"dbg_hd{l}", oT[:, :, CTXL:T], oT_r, [64, 4, SEQ], BF16)
        self.release(ms)
        wg, wg_r = self.alloc("wDg", 128, [KC, 256], BF16)
        self.load_w(wg, wg_r, d["w_in"][l][:, 3344:3600], 256)
        self.post_norm_gate(l, oT, oT_r, hnw[:, l, :], wg, wg_r, 0, bD[:, l, 16:20], AF.Silu, oT, oT_r)
        self.release(ms)
        return oT, oT_r, 64

    def wrap_pi(self, a, a_r, tmp, tmp_r, n):
        self.ts(tmp[:, 0:n], a[:, 0:n], PI, ALU.is_gt, rd=[a_r], wr=[tmp_r])
        self.stt(a[:, 0:n], tmp[:, 0:n], -2.0 * PI, a[:, 0:n], ALU.mult, ALU.add, rd=[tmp_r, a_r], wr=[a_r])
        self.ts(tmp[:, 0:n], a[:, 0:n], -PI, ALU.is_lt, rd=[a_r], wr=[tmp_r])
        self.stt(a[:, 0:n], tmp[:, 0:n], 2.0 * PI, a[:, 0:n], ALU.mult, ALU.add, rd=[tmp_r, a_r], wr=[a_r])
        self.ts(a[:, 0:n], a[:, 0:n], -3.141592, ALU.max, s2=3.141592, op1=ALU.min, rd=[a_r], wr=[a_r])

    def mixer_B(self, bi, l):
        d = self.dram
        last = (l == self.NL - 1)
        yT, yT_r = self.alloc("yB", 128, [2, T], BF16)
        ms = self.mark()
        x0c, x0c_r = self.alloc("x0c", 128, [2, T], BF16)
        uT, uT_r = self.alloc("uTB", 128, [2, T], BF16)
        m3 = self.mark()
        w, w_r = self.alloc("wB", 128, [KC, 768], BF16, hi=True)
        self.load_w(w, w_r, d["w_in"][l][:, 1040:1808], 768, hi=True)
        x1c, x1c_r = self.alloc("x1c", 128, [2, T])
        praw, praw_r = self.alloc("praw", 128, [T])
        pcv, pcv_r = self.alloc("pcv", 128, [T])
        hsw = self.P("hsw").rearrange("p (l j c) -> p l j c", l=2, j=3, c=6)
        hsb = self.P("hsb").rearrange("p (l c) -> p l c", l=2, c=6)
        bB = self.P("bB").rearrange("p (l c) -> p l c", l=2, c=6)
        seqs = [(CTXL, SEQ, "L")] if last else [(0, CTXL, "S"), (CTXL, SEQ, "L")]
        lo = CTXL if last else 0
        for c in range(6):
            for b, (t0, n) in enumerate(BLOCKS):
                if b == 0 and last:
                    continue
                ps, ps_r = self.inproj(w, w_r, c * 128, 128, b, t0, n)
                self.act(praw[:, t0:t0 + n], ps[:, 0:n], AF.Identity, rd=[ps_r, self.prm_r], wr=[praw_r], bias=bB[:, l, c:c + 1])
            for (s0, Ls, _) in seqs:
                e = s0 + Ls
                self.ts(pcv[:, s0:e], praw[:, s0:e], hsw[:, l, 1, c:c + 1], ALU.mult, s2=hsb[:, l, c:c + 1], op1=ALU.add,
                        rd=[praw_r, self.prm_r], wr=[pcv_r])
                self.stt(pcv[:, s0 + 1:e], praw[:, s0:e - 1], hsw[:, l, 0, c:c + 1], pcv[:, s0 + 1:e], ALU.mult, ALU.add,
                         rd=[praw_r, self.prm_r, pcv_r], wr=[pcv_r])
                self.stt(pcv[:, s0:e - 1], praw[:, s0 + 1:e], hsw[:, l, 2, c:c + 1], pcv[:, s0:e - 1], ALU.mult, ALU.add,
                         rd=[praw_r, self.prm_r, pcv_r], wr=[pcv_r])
            if c < 2:
                self.act(x0c[:, c, lo:T], pcv[:, lo:T], AF.Copy, rd=[pcv_r], wr=[x0c_r])
            elif c < 4:
                self.cp(x1c[:, c - 2, lo:T], pcv[:, lo:T], rd=[pcv_r], wr=[x1c_r], eng="pool")
            else:
                self.tt(uT[:, c - 4, lo:T], pcv[:, lo:T], x1c[:, c - 4, lo:T], ALU.mult, rd=[pcv_r, x1c_r], wr=[uT_r])
        self.release(m3, hi=True)
        for (s0, Ls, tag) in seqs:
            self.hy_conv(l, s0, Ls, tag, x0c, x0c_r, uT, uT_r, yT, yT_r)
        self.release(ms)
        return yT, yT_r, 128

    def hy_conv(self, l, s0, L, tag, x0c, x0c_r, uT, uT_r, yT, yT_r):
        d = self.dram
        mc = L // 128
        m0 = self.mark()
        RH, RH_r = self.alloc("RH", 128, [mc, 768], BF16)
        YS, YS_r = self.alloc("YS", 128, [2 * mc, 256], BF16)
        m1 = self.mark()
        zt, zt_r = self.alloc("zt", 17, [L])
        self.dma("sp", zt, d["z" + tag], wr=[zt_r])
        hd2, hd2_r = self.alloc("hd2", 64, [L])
        arg, arg_r = self.alloc("harg", 64, [512])
        tmpm, tmpm_r = self.alloc("htmp", 64, [512])
        hd1, hd1_r = self.alloc("hd1", 64, [512])
        fb, fb_r = self.alloc("hfb", 64, [2])
        f1 = self.P("hy_freq1")[:, l:l + 1]
        f2 = self.P("hy_freq2")[:, l:l + 1]
        self.tt(fb[:, 0:1], f1, self.P("hy_b1")[:, l:l + 1], ALU.mult, rd=[self.prm_r], wr=[fb_r])
        self.tt(fb[:, 1:2], f2, self.P("hy_b2")[:, l:l + 1], ALU.mult, rd=[self.prm_r], wr=[fb_r])
        hw1 = self.P("hw1").rearrange("p (l c) -> p l c", l=2, c=64)
        hw2 = self.P("hw2").rearrange("p (l c) -> p l c", l=2, c=64)
        hw3 = self.P("hw3").rearrange("p (l c) -> p l c", l=2, c=512)
        nb = min(512, L)
        for c0 in range(0, L, nb):
            ps, ps_r = self.PS()
            self.mm(ps[0:64, 0:nb], hw1[:, l, :], zt[:, c0:c0 + nb], rd=[self.prm_r, zt_r], wr=[ps_r])
            self.act(arg[:, 0:nb], ps[0:64, 0:nb], AF.Identity, rd=[ps_r, self.prm_r, fb_r], wr=[arg_r], bias=fb[:, 0:1], scale=f1)
            self.wrap_pi(arg, arg_r, tmpm, tmpm_r, nb)
            self.act(hd1[:, 0:nb], arg[:, 0:nb], AF.Sin, rd=[arg_r], wr=[hd1_r])
            ps, ps_r = self.PS()
            self.mm(ps[0:64, 0:nb], hw2[:, l, :], hd1[:, 0:nb], rd=[self.prm_r, hd1_r], wr=[ps_r])
            self.act(arg[:, 0:nb], ps[0:64, 0:nb], AF.Identity, rd=[ps_r, self.prm_r, fb_r], wr=[arg_r], bias=fb[:, 1:2], scale=f2)
            self.wrap_pi(arg, arg_r, tmpm, tmpm_r, nb)
            self.act(hd2[:, c0:c0 + nb], arg[:, 0:nb], AF.Sin, rd=[arg_r], wr=[hd2_r])
        dcs = [self.alloc(f"hdc{i}", 128, [2, 256]) for i in range(2)]
        hfs = [self.alloc(f"hf{i}", 128, [256]) for i in range(2)]
        hbs = [self.alloc(f"hb{i}", 128, [256]) for i in range(2)]
        for m in range(mc):
            dc, dc_r = dcs[m % 2]
            self.dma("sp", dc, d["dec" + tag][:, m], wr=[dc_r])
            ps, ps_r = self.PS()
            self.mm(ps[:, 0:512], hd2[:, m * 128:(m + 1) * 128], hw3[:, l, :], rd=[hd2_r, self.prm_r], wr=[ps_r])
            hf, hf_r = hfs[m % 2]
            hb, hb_r = hbs[m % 2]
            self.tt(hf, ps[:, 0:256], dc[:, 0, :], ALU.mult, rd=[ps_r, dc_r], wr=[hf_r])
            self.tt(hb, ps[:, 256:512], dc[:, 1, :], ALU.mult, rd=[ps_r, dc_r], wr=[hb_r])
            self.tt(RH[:, m, 0:256], hf, hb, ALU.add, rd=[hf_r, hb_r], wr=[RH_r.sub(m)], eng="pool")
            self.tt(RH[:, m, 512:768], hb, hf, ALU.subtract, rd=[hf_r, hb_r], wr=[RH_r.sub(m)], eng="pool")
            pb, pb_r = self.PSB()
            for cc in range(2):
                tk = s0 + m * 128
                self.tr(pb[:, cc * 128:(cc + 1) * 128], uT[:, cc, tk:tk + 128], self.ident_b, rd=[uT_r, self.cb_r], wr=[pb_r])
            self.cp(RH[:, m, 256:512], pb[:, 0:256], rd=[pb_r], wr=[RH_r.sub(m)])
        self.release(m1)
        m1 = self.mark()
        Fts = [self.alloc(f"Ft{i}", 128, [mc, 128], BF16) for i in range(4)]
        Kcs = [self.alloc(f"Kc{i}", 128, [256]) for i in range(2)]
        Kds = [self.alloc(f"Kd{i}", 128, [256]) for i in range(2)]
        Pt = [self.alloc(f"Pp{i}", 128, [256]) for i in range(4)]
        kny, kny_r = self.alloc("kny", 1, [256])
        for i in range(mc):
            fa, fa_r = Fts[(2 * i) % 4]
            fs, fs_r = Fts[(2 * i + 1) % 4]
            self.dma("sp", fa, d["F" + tag][i], wr=[fa_r])
            self.dma("sp", fs, d["F" + tag][mc + i], wr=[fs_r])
            pc, pc_r = self.PS()
            for m in range(mc):
                self.mm(pc[:, 0:512], fa[:, m, :], RH[:, m, 0:512], start=(m == 0), stop=(m == mc - 1), rd=[fa_r, RH_r], wr=[pc_r])
            pS, pS_r = self.PS()
            for m in range(mc):
                self.mm(pS[:, 0:512], fs[:, m, :], RH[:, m, 256:768], start=(m == 0), stop=(m == mc - 1), rd=[fs_r, RH_r], wr=[pS_r])
            if i == 0:
                pn, pn_r = self.PS()
                for m in range(mc):
                    self.mm(pn[:, 0:256], fs[:, m, :], RH[:, m, 0:256], start=(m == 0), stop=(m == mc - 1), rd=[fs_r, RH_r], wr=[pn_r])
            Kc, Kc_r = Kcs[i % 2]
            Kd, Kd_r = Kds[i % 2]
            self.act(Kc, pc[:, 0:256], AF.Copy, rd=[pc_r], wr=[Kc_r])
            self.act(Kd, pS[:, 256:512], AF.Copy, rd=[pS_r], wr=[Kd_r])
            (P1, P1_r), (P2, P2_r), (P3, P3_r), (P4, P4_r) = Pt
            self.tt(P1, pc[:, 256:512], Kc, ALU.mult, rd=[pc_r, Kc_r], wr=[P1_r])
            self.tt(P2, pS[:, 0:256], Kd, ALU.mult, rd=[pS_r, Kd_r], wr=[P2_r])
            self.tt(YS[:, i, :], P1, P2, ALU.add, rd=[P1_r, P2_r], wr=[YS_r.sub(i)], eng="pool")
            self.tt(P3, pc[:, 256:512], Kd, ALU.mult, rd=[pc_r, Kd_r], wr=[P3_r])
            self.tt(P4, pS[:, 0:256], Kc, ALU.mult, rd=[pS_r, Kc_r], wr=[P4_r])
            self.tt(YS[:, mc + i, :], P3, P4, ALU.subtract, rd=[P3_r, P4_r], wr=[YS_r.sub(mc + i)], eng="pool")
            if i == 0:
                self.cp(YS[0:1, 0, :], P1[0:1, :], rd=[P1_r, YS_r.sub(0)], wr=[YS_r.sub(0)], eng="pool")
                self.act(kny, pn[0:1, 0:256], AF.Copy, rd=[pn_r], wr=[kny_r])
                self.tt(YS[0:1, mc, :], pS[0:1, 0:256], kny, ALU.mult, rd=[pS_r, kny_r, YS_r.sub(mc)], wr=[YS_r.sub(mc)])
        self.release(m1)
        m1 = self.mark()
        n = min(512, L)
        q = min(8, 2 * mc)
        Gts = [self.alloc(f"Gt{i}", 128, [q, n], BF16) for i in range(3)]
        t1s = [self.alloc(f"ht1{i}", 128, [n]) for i in range(2)]
        dsk = self.P("hskip").rearrange("p (l c) -> p l c", l=2, c=2)
        gi = 0
        for tb in range(L // n):
            p0, p0_r = self.PS()
            p1, p1_r = self.PS()
            for qi in range(2 * mc // q):
                g, g_r = Gts[gi % 3]
                gi += 1
                self.dma("sp", g, d["G" + tag][tb][:, qi * q:(qi + 1) * q, :], wr=[g_r])
                for r in range(q):
                    fc = qi * q + r
                    self.mm(p0[:, 0:n], YS[:, fc, 0:128], g[:, r, :], start=(fc == 0), stop=(fc == 2 * mc - 1), rd=[YS_r, g_r], wr=[p0_r])
                    self.mm(p1[:, 0:n], YS[:, fc, 128:256], g[:, r, :], start=(fc == 0), stop=(fc == 2 * mc - 1), rd=[YS_r, g_r], wr=[p1_r])
            tk = s0 + tb * n
            for cc, (p, p_r) in enumerate(((p0, p0_r), (p1, p1_r))):
                t1, t1_r = t1s[cc]
                self.stt(t1[:, 0:n], uT[:, cc, tk:tk + n], dsk[:, l, cc:cc + 1], p[:, 0:n], ALU.mult, ALU.add, rd=[uT_r, self.prm_r, p_r], wr=[t1_r])
                self.tt(yT[:, cc, tk:tk + n], t1[:, 0:n], x0c[:, cc, tk:tk + n], ALU.mult, rd=[t1_r, x0c_r], wr=[yT_r])
        self.release(m0)


_PROG_CACHE = {}


def _get_prog(NB, NL, stages, dbg, prm_off, nprm):
    key = (NB, NL, stages, dbg)
    if key not in _PROG_CACHE:
        p = Prog(NB, NL, stages, dbg)
        off = dict(prm_off)
        off["__n__"] = nprm
        p.build(off, nprm)
        _PROG_CACHE[key] = p
    return _PROG_CACHE[key]


def make_in_maps(inp, n_cores, NB):
    P = _prep_shared(inp)
    prm = P.build()
    cst = _constants()
    f32 = lambda a: np.ascontiguousarray(np.asarray(a, np.float32))
    shared = {
        "prm": prm,
        "ada_w": f32(inp["ada_w"]), "w_in": f32(inp["w_in"]), "w_gate": f32(inp["w_gate"]),
        "w_branch": f32(inp["w_branch"]), "w_out": f32(inp["w_out"]), "w_ffn_in": f32(inp["w_ffn_in"]),
        "w_ffn_out": f32(inp["w_ffn_out"]),
        "gwsT": f32(np.transpose(np.asarray(inp["gm_ws"], np.float32), (3, 0, 1, 2))),
        "gbs": f32(np.asarray(inp["gm_bs"], np.float32).reshape(1, -1)),
    }
    for k, v in cst.items():
        shared[k] = v
    maps = []
    x = np.asarray(inp["x"], np.float32)
    ctx = np.asarray(inp["ctx"], np.float32)
    c = np.asarray(inp["c"], np.float32)
    cc = np.asarray(inp["c_ctx"], np.float32)
    for i in range(n_cores):
        b0 = i * NB
        m = dict(shared)
        m["xin"] = np.ascontiguousarray(x[b0:b0 + NB])
        m["cin"] = np.ascontiguousarray(ctx[b0:b0 + NB])
        cols = [_col(c[b0 + (j % NB)]) for j in range(2)] + [_col(cc)]
        m["csT"] = np.ascontiguousarray(np.stack(cols, axis=2))
        maps.append(m)
    return maps, P.off, prm.shape[1]


def kernel(**inp):
    n_cores, NB = 8, 2
    maps, off, nprm = make_in_maps(inp, n_cores, NB)
    prog = _get_prog(NB, 2, "ABCD", False, off, nprm)
    res = run_bass_kernel_spmd(prog.nc, maps, core_ids=list(range(n_cores)))
    out = np.concatenate([np.asarray(r["yout"], np.float32) for r in res.results], axis=0)
    return out
```

```python
import math, contextlib
import numpy as np
import ml_dtypes
import concourse.bass as bass
import concourse.mybir as mybir
from concourse.bass_utils import run_bass_kernel_spmd

F32 = mybir.dt.float32
BF16 = mybir.dt.bfloat16
AF = mybir.ActivationFunctionType
ALU = mybir.AluOpType

ENGS = ("pe", "act", "dve", "pool", "sp")
SEM_EPOCH = 20000

D = 1024
KC = 8
SEQ = 2048
CTXL = 256
T = SEQ + CTXL
NT = T // 128
BLOCKS = [(0, 256), (256, 512), (768, 512), (1280, 512), (1792, 512)]
FH = 2816
FCH = 22
EPS = 1e-6
PI = math.pi


class Res:
    __slots__ = ("name", "parent", "children", "w", "rs", "dma_sem", "dma_cnt")

    def __init__(self, name, parent=None):
        self.name = name
        self.parent = parent
        self.children = {}
        self.w = None
        self.rs = []
        self.dma_sem = None
        self.dma_cnt = 0

    def sub(self, key):
        r = self.children.get(key)
        if r is None:
            r = Res(f"{self.name}/{key}", self)
            self.children[key] = r
        return r

    def family(self):
        out = [self]
        p = self.parent
        while p is not None:
            out.append(p)
            p = p.parent
        stack = list(self.children.values())
        while stack:
            c = stack.pop()
            out.append(c)
            stack.extend(c.children.values())
        return out


class Rec:
    def __init__(self, nc):
        self.nc = nc
        self.ops = {e: [] for e in ENGS}
        self.cnt = {e: 0 for e in ENGS}
        self.seen = {e: {} for e in ENGS}
        self.semkeys = []
        self.semset = set()
        self.dma_res = []
        self.n_dma_sem = 0
        self.tags = {}
        self.free_sems = {}
        self.dcount = {}

    def _sem(self, key):
        if key not in self.semset:
            self.semset.add(key)
            self.semkeys.append(key)
        return key

    def _need(self, eng, dep, waits):
        if dep is None:
            return
        key, val, deng = dep
        if self.seen[eng].get(key, 0) >= val:
            return
        self.seen[eng][key] = val
        waits.append((key, val))

    def op(self, eng, fn, reads=(), writes=(), dma=False):
        waits = []
        for r in reads:
            for f in r.family():
                if f.w is not None:
                    if f.w[2] == eng and eng == "pe" and not dma and f.w[0][0] == "e":
                        continue
                    self._need(eng, f.w, waits)
        for w in writes:
            for f in w.family():
                if f.w is not None and not (f.w[2] == eng and eng == "pe" and not dma and f.w[0][0] == "e"):
                    self._need(eng, f.w, waits)
                for rd in f.rs:
                    if rd[2] == eng and eng == "pe" and not dma and rd[0][0] == "e":
                        continue
                    self._need(eng, rd, waits)
        if dma:
            dst = writes[0]
            if dst.dma_sem is None:
                fl = self.free_sems.setdefault(eng, [])
                if fl:
                    dst.dma_sem = fl.pop()
                else:
                    dst.dma_sem = self._sem(("d", eng, self.n_dma_sem))
                    self.n_dma_sem += 1
            assert dst.dma_sem[1] == eng, f"DMA semaphore of {dst.name} shared across queues"
            cntv = self.dcount.get(dst.dma_sem, 0) + 16
            self.dcount[dst.dma_sem] = cntv
            dst.dma_cnt = cntv
            done = (dst.dma_sem, cntv, eng)
            inc = (dst.dma_sem, 16)
        else:
            n = self.cnt[eng]
            self.cnt[eng] = n + 1
            ep = n // SEM_EPOCH
            key = self._sem(("e", eng, ep))
            done = (key, n - ep * SEM_EPOCH + 1, eng)
            inc = (key, 1)
        for w in writes:
            w.w = done
            w.rs = []
            stack = list(w.children.values())
            while stack:
                c = stack.pop()
                c.w = done
                c.rs = []
                stack.extend(c.children.values())
        for r in reads:
            r.rs.append(done)
            if len(r.rs) > 48:
                r.rs = r.rs[-48:]
        import sys as _sys
        fr = _sys._getframe(2)
        tag = f"{fr.f_code.co_name}:{fr.f_lineno}"
        fr2 = fr.f_back
        if fr2 is not None:
            tag += f"<{fr2.f_code.co_name}:{fr2.f_lineno}"
        self.ops[eng].append((waits, fn, inc, tag))
        return done

    def free_res(self, res):
        stack = [res]
        while stack:
            r = stack.pop()
            if r.dma_sem is not None:
                self.free_sems.setdefault(r.dma_sem[1], []).append(r.dma_sem)
                r.dma_sem = None
            stack.extend(r.children.values())

    def barrier(self):
        deps = []
        for e in ENGS:
            n = self.cnt[e]
            if n > 0:
                ep = (n - 1) // SEM_EPOCH
                deps.append((("e", e, ep), n - ep * SEM_EPOCH, e))
        for key, cntv in self.dcount.items():
            deps.append((key, cntv, "x"))
        for e in ENGS:
            waits = []
            for d in deps:
                self._need(e, d, waits)
            if waits:
                self.ops[e].append((waits, None, None, "barrier"))

    def emit(self):
        nc = self.nc
        with contextlib.ExitStack() as st:
            sems = {}
            for k in self.semkeys:
                sems[k] = st.enter_context(nc.semaphore("s_" + "_".join(str(x) for x in k)))
            block = st.enter_context(nc.Block())
            emap = {"pe": block.tensor, "act": block.scalar, "dve": block.vector,
                    "pool": block.gpsimd, "sp": block.sync}
            for e in ENGS:
                ops = self.ops[e]
                if not ops:
                    continue

                def body(eng, ops=ops):
                    for waits, fn, inc, tag in ops:
                        for (k, v) in waits:
                            eng.wait_ge(sems[k], v)
                        if fn is not None:
                            ins = fn(eng)
                            try:
                                self.tags[str(ins.ins.name)] = tag + ' | ' + str(ins.ins.concise())[:300]
                            except Exception:
                                pass
                            ins.then_inc(sems[inc[0]], inc[1])
                emap[e](body)


def _col(v, P=128):
    v = np.asarray(v, np.float32)
    return np.ascontiguousarray(v.reshape(-1, P).T)


class Pack:
    def __init__(self):
        self.items = []
        self.off = {}
        self.n = 0

    def add(self, name, arr):
        arr = np.asarray(arr, np.float32)
        if arr.ndim == 1:
            arr = arr[:, None]
        arr = arr.reshape(arr.shape[0], -1)
        self.off[name] = (self.n, arr.shape[0], arr.shape[1])
        self.items.append(arr)
        self.n += arr.shape[1]

    def build(self):
        out = np.zeros((128, self.n), np.float32)
        for (name, (o, p, c)), a in zip(self.off.items(), self.items):
            out[:p, o:o + c] = a
        return out


def _dft_tables(L):
    N = 2 * L
    m = np.arange(L, dtype=np.float64)[:, None]
    f = np.arange(L, dtype=np.float64)[None, :]
    ang = 2 * np.pi * ((m * f) % N) / N
    Fc = np.cos(ang)
    Fs = np.sin(ang)
    Fs[:, 0] = np.cos(np.pi * m[:, 0])
    F = np.concatenate([Fc, Fs], axis=1)
    wc = np.full((L, 1), 2.0 / N)
    wc[0, 0] = 1.0 / N
    Gc = wc * np.cos(ang.T)
    Gs = -(2.0 / N) * np.sin(ang.T)
    Gs[0, :] = np.cos(np.pi * m[:, 0]) / N
    G = np.concatenate([Gc, Gs], axis=0)
    return F.astype(np.float32), G.astype(np.float32)


def _hy_tables(L):
    t = np.linspace(0.0, 1.0, L, dtype=np.float32)[:, None]
    bands = np.linspace(1e-4, 7, 8, dtype=np.float32)[None]
    ang = (2 * np.float32(math.pi) * bands * np.arange(L, dtype=np.float32)[:, None] / np.float32(L)).astype(np.float32)
    z = np.concatenate([t, np.cos(ang), -np.sin(ang)], axis=-1).astype(np.float32)
    deltas = np.abs(np.linspace(math.log(1e-2) / 1.5, math.log(1e-2) / 0.3, 256, dtype=np.float32))
    decay = np.exp(-t * deltas[None]).astype(np.float32)
    decay0 = decay.copy()
    decay0[0] = 0.0
    return z, decay, decay0


_CONST_CACHE = {}


def _constants():
    if _CONST_CACHE:
        return _CONST_CACHE
    bf = ml_dtypes.bfloat16
    c = {}
    idx = np.arange(128)
    tri = (idx[:, None] <= idx[None, :]).astype(np.float32)
    ident = np.eye(128, dtype=np.float32)
    selrow = np.zeros((128, 4, 64), np.float32)
    for h in range(4):
        selrow[h, h, :] = 1.0
    pick = np.zeros((128, 2, 64), np.float32)
    pick[127, 0, :] = 1.0
    pick[0, 1, :] = 1.0
    c["cst_f"] = np.ascontiguousarray(np.concatenate([ident, tri, tri.T.copy(), selrow.reshape(128, 256), pick.reshape(128, 128)], axis=1))
    c["cst_b"] = np.ascontiguousarray(np.concatenate([ident, tri, tri.T.copy(), np.ones((128, 128), np.float32)], axis=1).astype(bf))
    rm = np.ones((64, 2, T), np.float32)
    rm[:, 0, 0::128] = 0.0
    rm[:, 1, 127::128] = 0.0
    c["rmask"] = rm.astype(bf)
    for L, tag in ((SEQ, "L"), (CTXL, "S")):
        F, G = _dft_tables(L)
        mc, fc = L // 128, (2 * L) // 128
        c["F" + tag] = np.ascontiguousarray(F.reshape(mc, 128, fc, 128).transpose(2, 1, 0, 3)).astype(bf)
        n = min(512, L)
        c["G" + tag] = np.ascontiguousarray(G.reshape(fc, 128, L // n, n).transpose(2, 1, 0, 3)).astype(bf)
        z, decay, decay0 = _hy_tables(L)
        c["z" + tag] = np.ascontiguousarray(z.T)
        dd = np.stack([decay, decay0], axis=1)
        c["dec" + tag] = np.ascontiguousarray(dd.reshape(mc, 128, 2, 256).transpose(1, 0, 2, 3))
    _CONST_CACHE.update(c)
    return c


def _prep_shared(inp):
    P = Pack()
    L2 = 2
    P.add("ada_bT", np.stack([_col(inp["ada_b"][l]) for l in range(L2)], 1))
    P.add("n1T", np.stack([_col(inp["norm1_w"][l]) for l in range(L2)], 1))
    P.add("n2T", np.stack([_col(inp["norm2_w"][l]) for l in range(L2)], 1))
    P.add("fnT", _col(inp["final_norm_w"]))
    b_in = inp["b_in"]
    P.add("bA", np.stack([_col(b_in[l, 0:1024], 64) for l in range(L2)], 1))
    P.add("bGrow", np.broadcast_to(b_in[None, :, 1024:1040], (128, 2, 16)))
    P.add("bB", np.stack([_col(b_in[l, 1040:1808]) for l in range(L2)], 1))
    P.add("bC", np.stack([_col(b_in[l, 1808:2320], 64) for l in range(L2)], 1))
    P.add("bD", np.stack([_col(b_in[l, 2320:3600], 64) for l in range(L2)], 1))
    P.add("mnw", np.stack([_col(inp["mlstm_norm_w"][l], 64) for l in range(L2)], 1))
    P.add("hnw", np.stack([_col(inp["hg_norm_w"][l], 64) for l in range(L2)], 1))
    P.add("gnw", np.stack([_col(inp["gm_norm_w"][l], 64) for l in range(L2)], 1))
    P.add("gnb", np.stack([_col(inp["gm_norm_b"][l], 64) for l in range(L2)], 1))
    P.add("lbz", np.stack([_col(inp["hg_lb_logits"][l], 64) for l in range(L2)], 1))
    P.add("hsw", np.stack([np.stack([_col(inp["hy_short_w"][l, j]) for j in range(3)], 1) for l in range(L2)], 1))
    P.add("hsb", np.stack([_col(inp["hy_short_b"][l]) for l in range(L2)], 1))
    P.add("hskip", np.stack([_col(inp["hy_bias"][l]) for l in range(L2)], 1))
    P.add("hw1", np.stack([inp["hy_w1"][l] for l in range(L2)], 1))
    P.add("hw2", np.stack([inp["hy_w2"][l] for l in range(L2)], 1))
    P.add("hw3", np.stack([inp["hy_w3"][l] for l in range(L2)], 1))
    for nm in ("hy_b1", "hy_freq1", "hy_b2", "hy_freq2"):
        P.add(nm, np.stack([inp[nm][l] for l in range(L2)], 1))
    return P


def _bf(a):
    return np.ascontiguousarray(np.asarray(a, np.float32).astype(ml_dtypes.bfloat16))


class Prog:
    def __init__(self, NB=2, NL=2, stages="ABCD", dbg=False):
        self.NB, self.NL, self.stages, self.dbg = NB, NL, stages, dbg
        self.nc = bass.Bass("TRN2", target_bir_lowering=False)
        self.R = Rec(self.nc)
        self.dram = {}
        self.dres = {}
        self.top = 0
        self.psi = 0
        self.psbi = 0
        self.dbg_outs = []

    def din(self, name, shape, dt=F32):
        self.dram[name] = self.nc.dram_tensor(name, list(shape), dt, kind="ExternalInput").ap()
        return self.dram[name]

    def dscratch(self, name, shape, dt=F32):
        self.dram[name] = self.nc.dram_tensor(name, list(shape), dt, kind="Internal").ap()
        self.dres[name] = Res(name)
        return self.dram[name]

    def alloc(self, name, P, free, dt=F32, hi=False):
        n = int(np.prod(free))
        words = n if dt == F32 else (n + 1) // 2
        words = (words + 7) // 8 * 8
        if not hasattr(self, "hi"):
            self.hi = self.AW
        assert self.top + words <= self.hi, f"SBUF arena overflow at {name}: {self.top}+{words}>{self.hi}"
        if hi:
            self.hi -= words
            base = self.hi
        else:
            base = self.top
        sl = self.arena[0:P, base:base + words]
        if dt != F32:
            sl = sl.bitcast(dt)
        sl = sl[:, 0:n]
        if len(free) == 2:
            sl = sl.rearrange("p (a b) -> p a b", a=free[0], b=free[1])
        elif len(free) == 3:
            sl = sl.rearrange("p (a b c) -> p a b c", a=free[0], b=free[1], c=free[2])
        if not hasattr(self, "alloc_log"):
            self.alloc_log = []
        _res = Res(name)
        self.alloc_log.append((base, hi, _res))
        if not hi:
            self.top += words
        self.peak = max(getattr(self, "peak", 0), self.top + (self.AW - self.hi))
        return sl, _res

    def mark(self):
        return self.top

    def release(self, m, hi=False):
        self.R.barrier()
        self.top = m
        if hi:
            self.hi = self.AW
        keep = []
        for (base, ishi, r) in getattr(self, "alloc_log", []):
            dead = (base >= self.hi) is False and ishi and hi
            if ishi:
                dead = hi
            else:
                dead = base >= m
            if dead:
                self.R.free_res(r)
            else:
                keep.append((base, ishi, r))
        self.alloc_log = keep

    def PS(self):
        i = self.psi
        self.psi = (i + 1) % len(self.psf)
        return self.psf[i], self.psf_res[i]

    def PSB(self):
        i = self.psbi
        self.psbi = (i + 1) % len(self.psb)
        return self.psb[i], self.psb_res[i]

    def mm(self, out, lhsT, rhs, start=True, stop=True, rd=(), wr=()):
        self.R.op("pe", lambda e: e.matmul(out, lhsT=lhsT, rhs=rhs, start=start, stop=stop), reads=rd, writes=wr)

    def tr(self, out, in_, ident, rd=(), wr=()):
        self.R.op("pe", lambda e: e.transpose(out=out, in_=in_, identity=ident), reads=rd, writes=wr)

    def act(self, out, in_, func, rd=(), wr=(), bias=None, scale=None):
        kw = {}
        if bias is not None:
            kw["bias"] = bias
        if scale is not None:
            kw["scale"] = scale
        self.R.op("act", lambda e: e.activation(out=out, in_=in_, func=func, **kw), reads=rd, writes=wr)

    def tt(self, out, a, b, op, rd=(), wr=(), eng="dve"):
        self.R.op(eng, lambda e: e.tensor_tensor(out=out, in0=a, in1=b, op=op), reads=rd, writes=wr)

    def ts(self, out, a, s1, op0, rd=(), wr=(), s2=None, op1=None, eng="dve"):
        if op1 is None:
            self.R.op(eng, lambda e: e.tensor_scalar(out=out, in0=a, scalar1=s1, scalar2=None, op0=op0), reads=rd, writes=wr)
        else:
            self.R.op(eng, lambda e: e.tensor_scalar(out=out, in0=a, scalar1=s1, scalar2=s2, op0=op0, op1=op1), reads=rd, writes=wr)

    def stt(self, out, a, s, b, op0, op1, rd=(), wr=()):
        self.R.op("dve", lambda e: e.scalar_tensor_tensor(out=out, in0=a, scalar=s, in1=b, op0=op0, op1=op1), reads=rd, writes=wr)

    def cp(self, out, in_, rd=(), wr=(), eng="dve"):
        self.R.op(eng, lambda e: e.tensor_copy(out=out, in_=in_), reads=rd, writes=wr)

    def recip(self, out, in_, rd=(), wr=()):
        self.R.op("dve", lambda e: e.reciprocal(out=out, in_=in_), reads=rd, writes=wr)

    def memset(self, ap, val, wr=(), eng="dve"):
        self.R.op(eng, lambda e: e.memset(ap, val), writes=wr)

    def scan(self, out, d0, d1, init, op0, op1, rd=(), wr=()):
        self.R.op("dve", lambda e: e.tensor_tensor_scan(out=out, data0=d0, data1=d1, initial=init, op0=op0, op1=op1), reads=rd, writes=wr)

    def dma(self, eng, out, in_, rd=(), wr=()):
        self.R.op(eng, lambda e: e.dma_start(out=out, in_=in_), reads=rd, writes=wr, dma=True)

    def dbg_dump(self, name, ap, res, shape, dt=F32):
        if not self.dbg:
            return
        o = self.nc.dram_tensor(name, list(shape), dt, kind="ExternalOutput").ap()
        self.dbg_outs.append(name)
        self.dma("sp", o, ap, rd=[res], wr=[Res("dbg_" + name)])

    def build(self, prm_off, NPRM):
        nc = self.nc
        NB, NL = self.NB, self.NL
        self.prm_off = prm_off
        xin = self.din("xin", [NB, SEQ, D])
        cin = self.din("cin", [NB, CTXL, D])
        self.din("csT", [128, KC, 3])
        self.din("prm", [128, NPRM])
        self.din("ada_w", [2, D, 6 * D])
        self.din("w_in", [2, D, 3600])
        self.din("w_gate", [2, 4, D, D])
        self.din("w_branch", [2, 4, 256, D])
        self.din("w_out", [2, D, D])
        self.din("w_ffn_in", [2, D, 2 * FH])
        self.din("w_ffn_out", [2, FH, D])
        self.din("gwsT", [128, 2, 4, 128])
        self.din("gbs", [1, 1024])
        self.din("cst_f", [128, 768])
        self.din("cst_b", [128, 512], BF16)
        self.din("rmask", [64, 2, T], BF16)
        for tag, L in (("L", SEQ), ("S", CTXL)):
            mc, fc = L // 128, 2 * L // 128
            n = min(512, L)
            self.din("F" + tag, [fc, 128, mc, 128], BF16)
            self.din("G" + tag, [L // n, 128, fc, n], BF16)
            self.din("z" + tag, [17, L])
            self.din("dec" + tag, [128, mc, 2, 256])
        yout = self.nc.dram_tensor("yout", [NB, SEQ, D], F32, kind="ExternalOutput").ap()
        self.yout = yout
        self.yres = Res("yout")
        park = self.dscratch("park", [128, KC, T])
        self.park, self.park_res = park, self.dres["park"]

        with contextlib.ExitStack() as st:
            self.AW = 53184
            self.arena = st.enter_context(nc.sbuf_tensor("arena", [128, self.AW], F32))
            self.psf = [st.enter_context(nc.psum_tensor(f"psf{i}", [128, 512], F32)) for i in range(6)]
            self.psb = [st.enter_context(nc.psum_tensor(f"psb{i}", [128, 1024], BF16)) for i in range(2)]
            self.psf_res = [Res(f"psf{i}") for i in range(6)]
            self.psb_res = [Res(f"psb{i}") for i in range(2)]
            self.body()
            self.R.barrier()
            self.R.emit()
        return nc

    def P(self, name):
        o, p, c = self.prm_off[name]
        return self.prm[0:p, o:o + c]

    def body(self):
        NB, NL = self.NB, self.NL
        d = self.dram
        self.prm, self.prm_r = self.alloc("prm", 128, [self.prm_off["__n__"]])
        self.dma("sp", self.prm, d["prm"], wr=[self.prm_r])
        cf, self.cf_r = self.alloc("cst_f", 128, [768])
        self.dma("sp", cf, d["cst_f"], wr=[self.cf_r])
        cb, self.cb_r = self.alloc("cst_b", 128, [512], BF16)
        self.dma("sp", cb, d["cst_b"], wr=[self.cb_r])
        self.ident_f, self.triF, self.triB = cf[:, 0:128], cf[:, 128:256], cf[:, 256:384]
        self.selrow = cf[0:4, 384:640].rearrange("p (h c) -> p h c", h=4, c=64)
        self.pick_last, self.pick_first = cf[:, 640:704], cf[:, 704:768]
        self.ident_b, self.maskF, self.maskB, self.ones_b = cb[:, 0:128], cb[:, 128:256], cb[:, 256:384], cb[:, 384:512]
        self.CR = [self.cf_r, self.cb_r, self.prm_r]
        cc, self.cc_r = self.alloc("consts", 128, [8])
        self.memset(cc[:, 0:1], EPS, wr=[self.cc_r])
        self.memset(cc[:, 1:2], -math.log(8.0), wr=[self.cc_r])
        self.memset(cc[:, 2:3], 1.0, wr=[self.cc_r])
        self.memset(cc[:, 3:4], 0.0, wr=[self.cc_r])
        self.c_eps, self.c_nl8, self.c_one, self.c_zero = cc[:, 0:1], cc[:, 1:2], cc[:, 2:3], cc[:, 3:4]
        self.CR.append(self.cc_r)
        for i in range(6):
            self.memset(self.psf[i][:, :], 0.0, wr=[self.psf_res[i]])
        self.mods()
        base = self.mark()
        for bi in range(NB):
            self.load_x(bi)
            for l in range(NL):
                self.layer(bi, l)
                if l == 0 and NL > 1:
                    self.permute(to_col=True)
            self.final(bi)
            self.release(base)

    def mods(self):
        d = self.dram
        NL = self.NL
        self.mod, self.mod_r = self.alloc("mod", 128, [2, 3, 48])
        self.drv, self.drv_r = self.alloc("drv", 128, [2, 3, 16])
        self.lb, self.lb_r = self.alloc("lb", 64, [2, 2, 4])
        m0 = self.mark()
        cs, cs_r = self.alloc("csS", 128, [KC, 3])
        self.dma("sp", cs, d["csT"], wr=[cs_r])
        self.act(cs, cs, AF.Silu, rd=[cs_r], wr=[cs_r])
        abuf = [self.alloc(f"adaw{i}", 128, [KC, 512]) for i in range(2)]
        for l in range(NL):
            ps, ps_r = self.PS()
            for ct in range(12):
                ab, ab_r = abuf[ct % 2]
                self.dma("sp", ab, d["ada_w"][l, :, ct * 512:(ct + 1) * 512].rearrange("(k p) c -> p k c", p=128), wr=[ab_r])
                for c4 in range(4):
                    c = ct * 4 + c4
                    for kc in range(KC):
                        self.mm(ps[:, c * 3:(c + 1) * 3], ab[:, kc, c4 * 128:(c4 + 1) * 128], cs[:, kc, :],
                                start=(kc == 0), stop=(kc == KC - 1), rd=[ab_r, cs_r], wr=[ps_r])
            abT = self.P("ada_bT").rearrange("p (l c) -> p l c", l=2, c=48)
            psv = ps[:, 0:144].rearrange("p (c j) -> p j c", c=48, j=3)
            for j in range(3):
                self.tt(self.mod[:, l, j, :], psv[:, j, :], abT[:, l, :], ALU.add, rd=[ps_r, self.prm_r], wr=[self.mod_r])
            n1 = self.P("n1T").rearrange("p (l c) -> p l c", l=2, c=8)
            n2 = self.P("n2T").rearrange("p (l c) -> p l c", l=2, c=8)
            for j in range(3):
                self.stt(self.drv[:, l, j, 0:8], self.mod[:, l, j, 8:16], 1.0, n1[:, l, :], ALU.add, ALU.mult,
                         rd=[self.mod_r, self.prm_r], wr=[self.drv_r])
                self.stt(self.drv[:, l, j, 8:16], self.mod[:, l, j, 32:40], 1.0, n2[:, l, :], ALU.add, ALU.mult,
                         rd=[self.mod_r, self.prm_r], wr=[self.drv_r])
        lz = self.P("lbz").rearrange("p (l h) -> p l h", l=2, h=4)
        self.memset(self.lb[:, 0, 0, :], 0.0, wr=[self.lb_r])
        self.memset(self.lb[:, 0, 1, :], 1.0, wr=[self.lb_r])
        self.tt(self.lb[:, 1, 0, :], lz[:, 1, :], lz[:, 0, :], ALU.subtract, rd=[self.prm_r], wr=[self.lb_r])
        self.act(self.lb[:, 1, 0, :], self.lb[:, 1, 0, :], AF.Sigmoid, rd=[self.lb_r], wr=[self.lb_r])
        self.ts(self.lb[:, 1, 1, :], self.lb[:, 1, 0, :], -1.0, ALU.mult, s2=1.0, op1=ALU.add, rd=[self.lb_r], wr=[self.lb_r])
        self.CR += [self.mod_r, self.drv_r, self.lb_r]
        self.dbg_dump("dbg_mod", self.mod[:, 0:NL], self.mod_r, [128, NL, 3, 48])
        self.release(m0)

    def load_x(self, bi):
        d = self.dram
        m = self.mark()
        stg = [self.alloc(f"xstg{i}", 128, [D]) for i in range(2)]
        xs = [self.alloc(f"xs{i}", 128, [KC, 128]) for i in range(2)]
        for t in range(NT):
            sb, sb_r = stg[t % 2]
            src = d["cin"][bi, t * 128:(t + 1) * 128, :] if t < 2 else d["xin"][bi, (t - 2) * 128:(t - 1) * 128, :]
            self.dma("sp", sb, src, wr=[sb_r])
            xo, xo_r = xs[t % 2]
            for half in range(2):
                ps, ps_r = self.PS()
                for q in range(4):
                    kc = half * 4 + q
                    self.tr(ps[:, q * 128:(q + 1) * 128], sb[:, kc * 128:(kc + 1) * 128], self.ident_f, rd=[sb_r, self.cf_r], wr=[ps_r])
                self.cp(xo[:, half * 4:(half + 1) * 4, :], ps[:, :].rearrange("p (k t) -> p k t", k=4, t=128), rd=[ps_r], wr=[xo_r],
                        eng="dve")
            self.dma("sp", self.park[:, :, t * 128:(t + 1) * 128], xo, rd=[xo_r], wr=[self.park_res.sub(t)])
        self.release(m)

    def park_subs(self, t0, n):
        return [self.park_res.sub(t) for t in range(t0 // 128, (t0 + n) // 128)]

    def norm_to_h(self, bi, l, which, hT, hT_r):
        xb = [self.alloc(f"nx{i}", 128, [KC, 512]) for i in range(2)]
        sq, sq_r = self.alloc("nsq", 128, [KC, 512], BF16)
        rs, rs_r = self.alloc("nrs", 128, [512])
        tmp = [self.alloc(f"ntmp{i}", 128, [512]) for i in range(2)]
        for b, (t0, n) in enumerate(BLOCKS):
            j = 2 if b == 0 else bi
            x, x_r = xb[b % 2]
            self.dma("sp", x[:, :, 0:n], self.park[:, :, t0:t0 + n], rd=self.park_subs(t0, n), wr=[x_r])
            self.act(sq[:, :, 0:n], x[:, :, 0:n], AF.Square, rd=[x_r], wr=[sq_r])
            ps, ps_r = self.PS()
            for kc in range(KC):
                self.mm(ps[:, 0:n], self.ones_b, sq[:, kc, 0:n], start=(kc == 0), stop=(kc == KC - 1), rd=[sq_r, self.cb_r], wr=[ps_r])
            self.act(rs[:, 0:n], ps[:, 0:n], AF.Ln, rd=[ps_r, self.cc_r], wr=[rs_r], bias=self.c_eps, scale=1.0 / D)
            self.act(rs[:, 0:n], rs[:, 0:n], AF.Exp, rd=[rs_r], wr=[rs_r], scale=-0.5)
            for kc in range(KC):
                tp, tp_r = tmp[kc % 2]
                self.tt(tp[:, 0:n], x[:, kc, 0:n], rs[:, 0:n], ALU.mult, rd=[x_r, rs_r], wr=[tp_r])
                A = self.drv[:, l, j, which * 8 + kc:which * 8 + kc + 1]
                mi = 0 if which == 0 else 3
                B = self.mod[:, l, j, mi * 8 + kc:mi * 8 + kc + 1]
                self.act(hT[:, kc, t0:t0 + n], tp[:, 0:n], AF.Identity, rd=[tp_r, self.drv_r, self.mod_r], wr=[hT_r.sub(b)], bias=B, scale=A)

    def load_w(self, dst, dst_r, src2d, ncols, rows=D, cast="act", hi=False, stage=None):
        kc_n = rows // 128
        cw = ncols if ncols <= 1100 else 1024
        if stage is None:
            stage = [self.alloc(f"wstg{i}", 128, [cw], hi=hi) for i in range(2)]
        k = getattr(self, "_stg_k", 0)
        for kc in range(kc_n):
            for c0 in range(0, ncols, cw):
                c1 = min(ncols, c0 + cw)
                st, st_r = stage[k % len(stage)]
                k += 1
                self.dma("sp", st[:, 0:c1 - c0], src2d[kc * 128:(kc + 1) * 128, c0:c1], wr=[st_r])
                if cast == "act":
                    self.act(dst[:, kc, c0:c1], st[:, 0:c1 - c0], AF.Copy, rd=[st_r], wr=[dst_r.sub(kc)])
                else:
                    self.cp(dst[:, kc, c0:c1], st[:, 0:c1 - c0], rd=[st_r], wr=[dst_r.sub(kc)], eng=cast)
        self._stg_k = k
        return stage

    def layer(self, bi, l):
        d = self.dram
        m0 = self.mark()
        hT, hT_r = self.alloc("hT", 128, [KC, T], BF16)
        acc, acc_r = self.alloc("acc", 128, [KC, T], BF16)
        self.hT, self.hT_r, self.acc, self.acc_r = hT, hT_r, acc, acc_r
        m1 = self.mark()
        self.norm_to_h(bi, l, 0, hT, hT_r)
        self.release(m1)
        first = True
        for j, name in enumerate("ABCD"):
            if name not in self.stages:
                continue
            m1 = self.mark()
            yT, yT_r, lay = getattr(self, "mixer_" + name)(bi, l)
            self.merge(l, j, yT, yT_r, lay, first)
            first = False
            self.release(m1)
        if first:
            self.memset(acc[:, :, :], 0.0, wr=[acc_r])
        m1 = self.mark()
        wo, wo_r = self.alloc("w_out", 128, [KC, D], BF16)
        self.load_w(wo, wo_r, d["w_out"][l], D)
        xb = [self.alloc(f"rx{i}", 128, [KC, 512]) for i in range(2)]
        for b, (t0, n) in enumerate(BLOCKS):
            if b == 0 and l == self.NL - 1:
                continue
            j = 2 if b == 0 else bi
            x, x_r = xb[b % 2]
            self.dma("sp", x[:, :, 0:n], self.park[:, :, t0:t0 + n], rd=self.park_subs(t0, n), wr=[x_r])
            for oc in range(KC):
                ps, ps_r = self.PS()
                for kc in range(KC):
                    self.mm(ps[:, 0:n], wo[:, kc, oc * 128:(oc + 1) * 128], acc[:, kc, t0:t0 + n], start=(kc == 0), stop=(kc == KC - 1),
                            rd=[wo_r.sub(kc), acc_r], wr=[ps_r])
                g = self.mod[:, l, j, 16 + oc:16 + oc + 1]
                self.stt(x[:, oc, 0:n], ps[:, 0:n], g, x[:, oc, 0:n], ALU.mult, ALU.add, rd=[ps_r, self.mod_r, x_r], wr=[x_r])
            self.dma("sp", self.park[:, :, t0:t0 + n], x[:, :, 0:n], rd=[x_r], wr=self.park_subs(t0, n))
        self.release(m0)
        self.ffn(bi, l)

    def merge(self, l, j, yT, yT_r, lay, first):
        d = self.dram
        wg, wg_r = self.alloc("w_gate", 128, [KC, D], BF16)
        self.load_w(wg, wg_r, d["w_gate"][l, j], D)
        bst = [self.alloc(f"wbstg{i}", 128, [D]) for i in range(2)]
        if lay == 64:
            wb, wb_r = self.alloc("w_br", 64, [4, D], BF16)
            for g in range(4):
                st, st_r = bst[g % 2]
                self.dma("sp", st[0:64, :], d["w_branch"][l, j, g * 64:(g + 1) * 64, :], wr=[st_r])
                self.act(wb[:, g, :], st[0:64, :], AF.Copy, rd=[st_r], wr=[wb_r.sub(g)])
            ng = 4
        else:
            wb, wb_r = self.alloc("w_br", 128, [2, D], BF16)
            for g in range(2):
                st, st_r = bst[g % 2]
                self.dma("sp", st, d["w_branch"][l, j, g * 128:(g + 1) * 128, :], wr=[st_r])
                self.act(wb[:, g, :], st, AF.Copy, rd=[st_r], wr=[wb_r.sub(g)])
            ng = 2
        sg = [self.alloc(f"sg{i}", 128, [512]) for i in range(2)]
        tm = [self.alloc(f"mt{i}", 128, [512], BF16) for i in range(2)]
        acc, acc_r, hT, hT_r = self.acc, self.acc_r, self.hT, self.hT_r
        it = 0
        for b, (t0, n) in enumerate(BLOCKS):
            if b == 0 and l == self.NL - 1:
                continue
            for oc in range(KC):
                ps, ps_r = self.PS()
                for kc in range(KC):
                    self.mm(ps[:, 0:n], wg[:, kc, oc * 128:(oc + 1) * 128], hT[:, kc, t0:t0 + n], start=(kc == 0), stop=(kc == KC - 1),
                            rd=[wg_r.sub(kc), hT_r.sub(b)], wr=[ps_r])
                s, s_r = sg[it % 2]
                self.act(s[:, 0:n], ps[:, 0:n], AF.Sigmoid, rd=[ps_r], wr=[s_r])
                ps2, ps2_r = self.PS()
                for g in range(ng):
                    self.mm(ps2[:, 0:n], wb[:, g, oc * 128:(oc + 1) * 128], yT[:, g, t0:t0 + n], start=(g == 0), stop=(g == ng - 1),
                            rd=[wb_r.sub(g), yT_r], wr=[ps2_r])
                if first:
                    self.tt(acc[:, oc, t0:t0 + n], ps2[:, 0:n], s[:, 0:n], ALU.mult, rd=[ps2_r, s_r], wr=[acc_r.sub(b)])
                else:
                    t_, t_r = tm[it % 2]
                    self.tt(t_[:, 0:n], ps2[:, 0:n], s[:, 0:n], ALU.mult, rd=[ps2_r, s_r], wr=[t_r])
                    self.tt(acc[:, oc, t0:t0 + n], acc[:, oc, t0:t0 + n], t_[:, 0:n], ALU.add, rd=[t_r, acc_r.sub(b)], wr=[acc_r.sub(b)], eng="pool")
                it += 1

    def ffn(self, bi, l):
        d = self.dram
        last = (l == self.NL - 1)
        m0 = self.mark()
        h2, h2_r = self.alloc("h2T", 128, [KC, T], BF16, hi=True)
        m1 = self.mark()
        self.norm_to_h(bi, l, 1, h2, h2_r)
        self.release(m1)
        hid, hid_r = self.alloc("hid", 128, [FCH, T], BF16)
        m1 = self.mark()
        wa = [self.alloc(f"wa{i}", 128, [KC, 256], BF16) for i in range(2)]
        wgt = [self.alloc(f"wg{i}", 128, [KC, 256], BF16) for i in range(2)]
        sl = [self.alloc(f"fsl{i}", 128, [512]) for i in range(2)]
        it = 0
        fstg = [self.alloc(f"fstg{i}", 128, [256]) for i in range(4)]

        def ld(cg):
            a_w, a_r = wa[cg % 2]
            g_w, g_r = wgt[cg % 2]
            self.load_w(a_w, a_r, d["w_ffn_in"][l][:, cg * 256:(cg + 1) * 256], 256, cast="pool", stage=fstg)
            self.load_w(g_w, g_r, d["w_ffn_in"][l][:, FH + cg * 256:FH + (cg + 1) * 256], 256, cast="pool", stage=fstg)
        ld(0)
        for cg in range(FCH // 2):
            a_w, a_r = wa[cg % 2]
            g_w, g_r = wgt[cg % 2]
            if cg + 1 < FCH // 2:
                ld(cg + 1)
            for c2 in range(2):
                c = cg * 2 + c2
                for b, (t0, n) in enumerate(BLOCKS):
                    if b == 0 and last:
                        continue
                    pa, pa_r = self.PS()
                    for kc in range(KC):
                        self.mm(pa[:, 0:n], a_w[:, kc, c2 * 128:(c2 + 1) * 128], h2[:, kc, t0:t0 + n], start=(kc == 0), stop=(kc == KC - 1),
                                rd=[a_r.sub(kc), h2_r.sub(b)], wr=[pa_r])
                    pg, pg_r = self.PS()
                    for kc in range(KC):
                        self.mm(pg[:, 0:n], g_w[:, kc, c2 * 128:(c2 + 1) * 128], h2[:, kc, t0:t0 + n], start=(kc == 0), stop=(kc == KC - 1),
                                rd=[g_r.sub(kc), h2_r.sub(b)], wr=[pg_r])
                    s, s_r = sl[it % 2]
                    it += 1
                    self.act(s[:, 0:n], pa[:, 0:n], AF.Silu, rd=[pa_r], wr=[s_r])
                    self.tt(hid[:, c, t0:t0 + n], pg[:, 0:n], s[:, 0:n], ALU.mult, rd=[pg_r, s_r], wr=[hid_r.sub(b)])
        self.release(m1, hi=True)
        wd, wd_r = self.alloc("w_down", 128, [FCH, D], BF16)
        self.load_w(wd, wd_r, d["w_ffn_out"][l], D, rows=FH)
        xb = [self.alloc(f"fx{i}", 128, [KC, 512]) for i in range(2)]
        for b, (t0, n) in enumerate(BLOCKS):
            if b == 0 and last:
                continue
            j = 2 if b == 0 else bi
            x, x_r = xb[b % 2]
            self.dma("sp", x[:, :, 0:n], self.park[:, :, t0:t0 + n], rd=self.park_subs(t0, n), wr=[x_r])
            for oc in range(KC):
                ps, ps_r = self.PS()
                for c in range(FCH):
                    self.mm(ps[:, 0:n], wd[:, c, oc * 128:(oc + 1) * 128], hid[:, c, t0:t0 + n], start=(c == 0), stop=(c == FCH - 1),
                            rd=[wd_r.sub(c), hid_r.sub(b)], wr=[ps_r])
                g = self.mod[:, l, j, 40 + oc:40 + oc + 1]
                self.stt(x[:, oc, 0:n], ps[:, 0:n], g, x[:, oc, 0:n], ALU.mult, ALU.add, rd=[ps_r, self.mod_r, x_r], wr=[x_r])
            self.dma("sp", self.park[:, :, t0:t0 + n], x[:, :, 0:n], rd=[x_r], wr=self.park_subs(t0, n))
        self.release(m0)

    def permute(self, to_col):
        m0 = self.mark()
        a = [self.alloc(f"pa{i}", 128, [SEQ]) for i in range(2)]
        bb = [self.alloc(f"pb{i}", 128, [SEQ]) for i in range(2)]
        lat = self.park_subs(CTXL, SEQ)
        for kc in range(KC):
            x, x_r = a[kc % 2]
            y, y_r = bb[kc % 2]
            self.dma("sp", x, self.park[:, kc, CTXL:T], rd=lat, wr=[x_r])
            if to_col:
                src = x.rearrange("p (r c) -> p c r", r=32, c=64)
                dst = y.rearrange("p (c r) -> p c r", c=64, r=32)
            else:
                src = x.rearrange("p (c r) -> p r c", c=64, r=32)
                dst = y.rearrange("p (r c) -> p r c", r=32, c=64)
            self.cp(dst, src, rd=[x_r], wr=[y_r], eng="pool")
            self.dma("sp", self.park[:, kc, CTXL:T], y, rd=[y_r], wr=lat)
        self.release(m0)

    def final(self, bi):
        if self.NL > 1:
            self.permute(to_col=False)
        m0 = self.mark()
        xb = [self.alloc(f"ox{i}", 128, [KC, 512]) for i in range(2)]
        sq, sq_r = self.alloc("osq", 128, [KC, 512], BF16)
        rs, rs_r = self.alloc("ors", 128, [512])
        xn, xn_r = self.alloc("oxn", 128, [KC, 512])
        ot = [self.alloc(f"ot{i}", 128, [D]) for i in range(2)]
        fn = self.P("fnT")
        it = 0
        for b, (t0, n) in enumerate(BLOCKS):
            if b == 0:
                continue
            x, x_r = xb[b % 2]
            self.dma("sp", x[:, :, 0:n], self.park[:, :, t0:t0 + n], rd=self.park_subs(t0, n), wr=[x_r])
            self.act(sq[:, :, 0:n], x[:, :, 0:n], AF.Square, rd=[x_r], wr=[sq_r])
            ps, ps_r = self.PS()
            for kc in range(KC):
                self.mm(ps[:, 0:n], self.ones_b, sq[:, kc, 0:n], start=(kc == 0), stop=(kc == KC - 1), rd=[sq_r, self.cb_r], wr=[ps_r])
            self.act(rs[:, 0:n], ps[:, 0:n], AF.Ln, rd=[ps_r, self.cc_r], wr=[rs_r], bias=self.c_eps, scale=1.0 / D)
            self.act(rs[:, 0:n], rs[:, 0:n], AF.Exp, rd=[rs_r], wr=[rs_r], scale=-0.5)
            for kc in range(KC):
                self.stt(xn[:, kc, 0:n], x[:, kc, 0:n], fn[:, kc:kc + 1], rs[:, 0:n], ALU.mult, ALU.mult, rd=[x_r, rs_r, self.prm_r], wr=[xn_r])
            for tl in range(n // 128):
                o, o_r = ot[it % 2]
                it += 1
                for half in range(2):
                    ps, ps_r = self.PS()
                    for q in range(4):
                        kc = half * 4 + q
                        self.tr(ps[:, q * 128:(q + 1) * 128], xn[:, kc, tl * 128:(tl + 1) * 128], self.ident_f, rd=[xn_r, self.cf_r], wr=[ps_r])
                    self.act(o[:, half * 512:(half + 1) * 512], ps[:, :], AF.Copy, rd=[ps_r], wr=[o_r])
                tok = t0 - CTXL + tl * 128
                self.dma("sp", self.yout[bi, tok:tok + 128, :], o, rd=[o_r], wr=[self.yres])
        self.release(m0)

    def inproj(self, w, w_r, col, M, b, t0, n):
        ps, ps_r = self.PS()
        for kc in range(KC):
            self.mm(ps[0:M, 0:n], w[:, kc, col:col + M], self.hT[:, kc, t0:t0 + n], start=(kc == 0), stop=(kc == KC - 1),
                    rd=[w_r.sub(kc), self.hT_r.sub(b)], wr=[ps_r])
        return ps, ps_r

    def post_norm_gate(self, l, oT, oT_r, nw, w, w_r, gcol, gbias, func, yT, yT_r):
        sq, sq_r = self.alloc("pn_sq", 64, [4, 512], BF16)
        rs, rs_r = self.alloc("pn_rs", 64, [512])
        gs, gs_r = self.alloc("pn_gs", 64, [512])
        tp, tp_r = self.alloc("pn_tp", 64, [512])
        for b, (t0, n) in enumerate(BLOCKS):
            if b == 0 and l == self.NL - 1:
                continue
            self.act(sq[:, :, 0:n], oT[:, :, t0:t0 + n], AF.Square, rd=[oT_r], wr=[sq_r])
            for h in range(4):
                ps, ps_r = self.PS()
                self.mm(ps[0:64, 0:n], self.ones_b[0:64, 0:64], sq[:, h, 0:n], rd=[sq_r, self.cb_r], wr=[ps_r])
                self.act(rs[:, 0:n], ps[0:64, 0:n], AF.Ln, rd=[ps_r, self.cc_r], wr=[rs_r], bias=self.c_eps[0:64, :], scale=1.0 / 64)
                self.act(rs[:, 0:n], rs[:, 0:n], AF.Exp, rd=[rs_r], wr=[rs_r], scale=-0.5)
                pg, pg_r = self.inproj(w, w_r, gcol + h * 64, 64, b, t0, n)
                self.act(gs[:, 0:n], pg[0:64, 0:n], func, rd=[pg_r, self.prm_r], wr=[gs_r], bias=gbias[:, h:h + 1])
                self.tt(tp[:, 0:n], oT[:, h, t0:t0 + n], rs[:, 0:n], ALU.mult, rd=[oT_r, rs_r], wr=[tp_r])
                self.stt(yT[:, h, t0:t0 + n], tp[:, 0:n], nw[:, h:h + 1], gs[:, 0:n], ALU.mult, ALU.mult, rd=[tp_r, gs_r, self.prm_r], wr=[yT_r])

    def mixer_C(self, bi, l):
        d = self.dram
        last = (l == self.NL - 1)
        yT, yT_r = self.alloc("yC", 64, [4, T], BF16)
        ms = self.mark()
        w, w_r = self.alloc("wC", 128, [KC, 512], BF16)
        self.load_w(w, w_r, d["w_in"][l][:, 1808:2320], 512)
        gw, gw_r = self.alloc("gw", 128, [4, 128], BF16)
        self.dma("pool", gw, d["gwsT"][:, l], wr=[gw_r])
        gb, gb_r = self.alloc("gb", 1, [512], BF16)
        self.dma("pool", gb, d["gbs"][0:1, l * 512:(l + 1) * 512], wr=[gb_r])
        bC = self.P("bC").rearrange("p (l g) -> p l g", l=2, g=8)
        gnw = self.P("gnw").rearrange("p (l g) -> p l g", l=2, g=4)
        gnb = self.P("gnb").rearrange("p (l g) -> p l g", l=2, g=4)
        vf, vf_r = self.alloc("vf", 64, [4, T])
        vn, vn_r = self.alloc("vn", 64, [4, T], BF16)
        vbs = [self.alloc(f"vb{i}", 64, [4, 512], BF16) for i in range(2)]
        vqs = [self.alloc(f"vq{i}", 64, [4, 512], BF16) for i in range(2)]
        mus = [self.alloc(f"mu{i}", 64, [512]) for i in range(2)]
        vas = [self.alloc(f"va{i}", 64, [512]) for i in range(2)]
        tqs = [self.alloc(f"tq{i}", 64, [512]) for i in range(4)]
        vts = [self.alloc(f"vt{i}", 128, [4, 64], BF16) for i in range(3)]
        blocks = [(b, t0, n) for b, (t0, n) in enumerate(BLOCKS) if not (b == 0 and last)]
        for (b, t0, n) in blocks:
            for g in range(8):
                ps, ps_r = self.inproj(w, w_r, g * 64, 64, b, t0, n)
                if g < 4:
                    self.act(yT[:, g, t0:t0 + n], ps[0:64, 0:n], AF.Gelu_apprx_tanh, rd=[ps_r, self.prm_r], wr=[yT_r.sub(b)], bias=bC[:, l, g:g + 1])
                else:
                    self.act(vf[:, g - 4, t0:t0 + n], ps[0:64, 0:n], AF.Gelu_apprx_tanh, rd=[ps_r, self.prm_r], wr=[vf_r.sub(b)], bias=bC[:, l, g:g + 1])
        for i, (b, t0, n) in enumerate(blocks):
            vb, vb_r = vbs[i % 2]
            vq, vq_r = vqs[i % 2]
            mu, mu_r = mus[i % 2]
            va, va_r = vas[i % 2]
            self.cp(vb[:, :, 0:n], vf[:, :, t0:t0 + n], rd=[vf_r.sub(b)], wr=[vb_r], eng="pool")
            self.act(vq[:, :, 0:n], vf[:, :, t0:t0 + n], AF.Square, rd=[vf_r.sub(b)], wr=[vq_r])
            pm, pm_r = self.PS()
            for g in range(4):
                self.mm(pm[0:64, 0:n], self.ones_b[0:64, 0:64], vb[:, g, 0:n], start=(g == 0), stop=(g == 3), rd=[vb_r, self.cb_r], wr=[pm_r])
            pq, pq_r = self.PS()
            for g in range(4):
                self.mm(pq[0:64, 0:n], self.ones_b[0:64, 0:64], vq[:, g, 0:n], start=(g == 0), stop=(g == 3), rd=[vq_r, self.cb_r], wr=[pq_r])
            tq, tq_r = tqs[0]
            self.act(mu[:, 0:n], pm[0:64, 0:n], AF.Copy, rd=[pm_r], wr=[mu_r], scale=1.0 / 256)
            self.tt(tq[:, 0:n], mu[:, 0:n], mu[:, 0:n], ALU.mult, rd=[mu_r], wr=[tq_r])
            self.stt(va[:, 0:n], pq[0:64, 0:n], 1.0 / 256, tq[:, 0:n], ALU.mult, ALU.subtract, rd=[pq_r, tq_r], wr=[va_r])
            self.act(va[:, 0:n], va[:, 0:n], AF.Ln, rd=[va_r, self.cc_r], wr=[va_r], bias=self.c_eps[0:64, :])
            self.act(va[:, 0:n], va[:, 0:n], AF.Exp, rd=[va_r], wr=[va_r], scale=-0.5)
            for g in range(4):
                tq, tq_r = tqs[g]
                self.tt(tq[:, 0:n], vf[:, g, t0:t0 + n], mu[:, 0:n], ALU.subtract, rd=[vf_r.sub(b), mu_r], wr=[tq_r])
                self.tt(tq[:, 0:n], tq[:, 0:n], va[:, 0:n], ALU.mult, rd=[tq_r, va_r], wr=[tq_r])
            for g in range(4):
                tq, tq_r = tqs[g]
                self.act(vn[:, g, t0:t0 + n], tq[:, 0:n], AF.Identity, rd=[tq_r, self.prm_r], wr=[vn_r.sub(b)], bias=gnb[:, l, g:g + 1], scale=gnw[:, l, g:g + 1])
        it = 0
        for (b, t0, n) in blocks:
            for tl in range(n // 128):
                tk = t0 + tl * 128
                pb, pb_r = self.PSB()
                for g in range(4):
                    self.tr(pb[:, g * 64:(g + 1) * 64], vn[:, g, tk:tk + 128], self.ident_b[0:64, 0:64], rd=[vn_r.sub(b), self.cb_r], wr=[pb_r])
                vt, vt_r = vts[it % 3]
                it += 1
                self.act(vt, pb[:, 0:256].rearrange("p (g c) -> p g c", g=4, c=64), AF.Copy, rd=[pb_r], wr=[vt_r])
                ps, ps_r = self.PS()
                for g in range(4):
                    gs = slice(g * 128, (g + 1) * 128)
                    self.mm(ps[0:64, gs], vt[:, g, :], gw[:, g, :], start=True, stop=False, rd=[vt_r, gw_r], wr=[ps_r])
                    self.mm(ps[0:64, gs], self.ones_b[0:1, 0:64], gb[0:1, g * 128:(g + 1) * 128], start=False, stop=True, rd=[gb_r, self.cb_r], wr=[ps_r])
                self.tt(yT[:, :, tk:tk + 128], ps[0:64, 0:512].rearrange("p (g t) -> p g t", g=4, t=128), yT[:, :, tk:tk + 128], ALU.mult,
                        rd=[ps_r, yT_r.sub(b)], wr=[yT_r.sub(b)])
        self.release(ms)
        return yT, yT_r, 64

    def mixer_A(self, bi, l):
        d = self.dram
        last = (l == self.NL - 1)
        ha, ha_r = self.alloc("ha", 64, [4, T], BF16)
        yT, yT_r = ha, ha_r
        ms = self.mark()
        m2 = self.mark()
        w, w_r = self.alloc("wA", 128, [KC, 1040], BF16, hi=True)
        self.load_w(w, w_r, d["w_in"][l][:, 0:1040], 1040, hi=True)
        bA = self.P("bA").rearrange("p (l g) -> p l g", l=2, g=16)
        bG = self.P("bGrow").rearrange("p (l g) -> p l g", l=2, g=16)
        mnw = self.P("mnw").rearrange("p (l g) -> p l g", l=2, g=4)
        qk, qk_r = self.alloc("qk", 64, [8, T], BF16)
        ktok, ktok_r = self.alloc("ktok", 128, [NT, 256], BF16)
        vones, vones_r = self.alloc("vtok", 128, [NT, 4, 64], BF16)
        gtok, gtok_r = self.alloc("gtok", 128, [NT, 16], hi=True)
        alpha, alpha_r = self.alloc("alpha", 128, [NT, 8])
        rows, rows_r = self.alloc("rows", 4, [2, T])
        vtmp, vtmp_r = self.alloc("vtmp", 64, [4, 512], BF16, hi=True)
        for b, (t0, n) in enumerate(BLOCKS):
            for g in range(12):
                ps, ps_r = self.inproj(w, w_r, g * 64, 64, b, t0, n)
                if g < 8:
                    self.act(qk[:, g, t0:t0 + n], ps[0:64, 0:n], AF.Identity, rd=[ps_r, self.prm_r], wr=[qk_r], bias=bA[:, l, g:g + 1])
                else:
                    self.act(vtmp[:, g - 8, 0:n], ps[0:64, 0:n], AF.Identity, rd=[ps_r, self.prm_r], wr=[vtmp_r], bias=bA[:, l, g:g + 1])
            for tl in range(n // 128):
                t = t0 // 128 + tl
                pb, pb_r = self.PSB()
                for h in range(4):
                    self.tr(pb[:, h * 64:(h + 1) * 64], qk[:, 4 + h, t * 128:(t + 1) * 128], self.ident_b[0:64, 0:64], rd=[qk_r, self.cb_r], wr=[pb_r])
                    self.tr(pb[:, 256 + h * 64:256 + (h + 1) * 64], vtmp[:, h, tl * 128:(tl + 1) * 128], self.ident_b[0:64, 0:64], rd=[vtmp_r, self.cb_r], wr=[pb_r])
                self.cp(ktok[:, t, :], pb[:, 0:256], rd=[pb_r], wr=[ktok_r])
                self.cp(vones[:, t, :, :], pb[:, 256:512].rearrange("p (h c) -> p h c", h=4, c=64), rd=[pb_r], wr=[vones_r])
                pg, pg_r = self.PS()
                for kc in range(KC):
                    self.mm(pg[:, 0:16], self.hT[:, kc, t * 128:(t + 1) * 128], w[:, kc, 1024:1040], start=(kc == 0), stop=(kc == KC - 1),
                            rd=[w_r.sub(kc), self.hT_r.sub(b)], wr=[pg_r])
                self.tt(gtok[:, t, :], pg[:, 0:16], bG[:, l, :], ALU.add, rd=[pg_r, self.prm_r], wr=[gtok_r])
        dcall, dcall_r = self.alloc("dcall", 64, [NT, 8])
        cstk = [self.alloc(f"cstk{i}", 128, [8]) for i in range(2)]
        g4 = gtok.rearrange("p t (a c) -> p t a c", a=4, c=4)
        for a in (1, 3):
            self.act(g4[:, :, a, :], g4[:, :, a, :], AF.Exp, rd=[gtok_r], wr=[gtok_r], scale=-1.0)
            self.act(g4[:, :, a, :], g4[:, :, a, :], AF.Ln, rd=[gtok_r, self.cc_r], wr=[gtok_r], bias=self.c_one)
        for t in range(NT):
            pc, pc_r = self.PS()
            self.mm(pc[:, 0:4], self.triF, gtok[:, t, 4:8], rd=[gtok_r, self.cf_r], wr=[pc_r])
            self.mm(pc[:, 4:8], self.triB, gtok[:, t, 12:16], rd=[gtok_r, self.cf_r], wr=[pc_r])
            self.mm(pc[0:4, 128:256], gtok[:, t, 4:8], self.triF, rd=[gtok_r, self.cf_r], wr=[pc_r])
            self.mm(pc[0:4, 256:384], gtok[:, t, 12:16], self.triB, rd=[gtok_r, self.cf_r], wr=[pc_r])
            self.tt(alpha[:, t, 0:4], pc[:, 0:4], gtok[:, t, 0:4], ALU.add, rd=[pc_r, gtok_r], wr=[alpha_r])
            self.tt(alpha[:, t, 4:8], pc[:, 4:8], gtok[:, t, 8:12], ALU.add, rd=[pc_r, gtok_r], wr=[alpha_r])
            self.cp(rows[:, 0, t * 128:(t + 1) * 128], pc[0:4, 128:256], rd=[pc_r], wr=[rows_r])
            self.cp(rows[:, 1, t * 128:(t + 1) * 128], pc[0:4, 256:384], rd=[pc_r], wr=[rows_r])
            ck, ck_r = cstk[t % 2]
            self.act(ck, pc[:, 0:8], AF.Copy, rd=[pc_r], wr=[ck_r])
            pe2, pe2_r = self.PS()
            self.mm(pe2[0:64, 0:4], self.pick_last, ck[:, 0:4], rd=[ck_r, self.cf_r], wr=[pe2_r])
            self.mm(pe2[0:64, 4:8], self.pick_first, ck[:, 4:8], rd=[ck_r, self.cf_r], wr=[pe2_r])
            self.act(dcall[:, t, :], pe2[0:64, 0:8], AF.Exp, rd=[pe2_r], wr=[dcall_r], scale=-1.0)
        self.act(alpha[:, :, :], alpha[:, :, :], AF.Exp, rd=[alpha_r, self.cc_r], wr=[alpha_r], bias=self.c_nl8)
        self.R.barrier()
        self.hi = self.AW
        Cst = [self.alloc(f"Cst{i}", 64, [4, 128]) for i in range(2)]
        Cbf = [[self.alloc(f"Cbf{i}{p}", 64, [4, 128], BF16) for p in range(2)] for i in range(2)]
        for i in range(2):
            self.memset(Cst[i][0][:, :, :], 0.0, wr=[Cst[i][1]])
            self.memset(Cbf[i][0][0][:, :, :], 0.0, wr=[Cbf[i][0][1]])
        vps = [self.alloc(f"vp{i}", 128, [4, 128], BF16) for i in range(3)]
        WTs = [self.alloc(f"WT{i}", 128, [4, 128], BF16) for i in range(3)]
        Ens = [self.alloc(f"En{i}", 64, [4, 128]) for i in range(2)]
        aYs = [self.alloc(f"aY{i}", 64, [4, 128]) for i in range(2)]
        hts = [self.alloc(f"hb{i}", 64, [4, 128], BF16) for i in range(2)]
        tcs = [self.alloc(f"tc{i}", 64, [4, 128]) for i in range(2)]
        orders = [list(range(NT)), [1, 0] + list(range(NT - 1, 1, -1))]
        written = set()
        it = 0
        LB = [(self.psf[i], self.psf_res[i]) for i in range(4)]
        SB = [(self.psf[i], self.psf_res[i]) for i in (4, 5)]
        li = [0]
        si = [0]

        def nextL():
            li[0] += 1
            return LB[li[0] % 4]

        def nextS():
            si[0] += 1
            return SB[si[0] % 2]

        def v4(ap):
            return ap.rearrange("p (h t) -> p h t", h=4, t=128)
        pending = None

        def tail(args):
            t, px, px_r, aY, aY_r, En, En_r, k = args
            tsl = slice(t * 128, (t + 1) * 128)
            self.tt(aY, aY, En, ALU.max, rd=[aY_r, En_r], wr=[aY_r])
            self.recip(aY, aY, rd=[aY_r], wr=[aY_r])
            hview = ha[:, :, tsl]
            if t not in written:
                written.add(t)
                self.tt(hview, v4(px[0:64, 0:512]), aY, ALU.mult, rd=[px_r, aY_r], wr=[ha_r.sub(t)])
            else:
                hb, hb_r = hts[k % 2]
                self.tt(hb, v4(px[0:64, 0:512]), aY, ALU.mult, rd=[px_r, aY_r], wr=[hb_r])
                self.tt(hview, hview, hb, ALU.add, rd=[hb_r, ha_r.sub(t)], wr=[ha_r.sub(t)], eng="pool")
        for sidx in range(NT):
            for dd in range(2):
                t = orders[dd][sidx]
                it += 1
                mask = self.maskF if dd == 0 else self.maskB
                skip_out = last and t < 2
                tsl = slice(t * 128, (t + 1) * 128)
                a_b = alpha[:, t, dd * 4:dd * 4 + 4].unsqueeze(2).to_broadcast([128, 4, 64])
                cs_, cs_r_ = Cst[dd]
                cb_cur, cb_cur_r = Cbf[dd][sidx % 2]
                cb_nxt, cb_nxt_r = Cbf[dd][(sidx + 1) % 2]
                vp, vp_r = vps[it % 3]
                self.tt(vp[:, :, 0:64], vones[:, t, :, :], a_b, ALU.mult, rd=[vones_r, alpha_r], wr=[vp_r], eng="pool")
                self.cp(vp[:, :, 64:128], a_b, rd=[alpha_r], wr=[vp_r], eng="pool")
                pu, pu_r = nextS()
                for h in range(4):
                    self.mm(pu[0:64, h * 128:(h + 1) * 128], ktok[:, t, h * 64:(h + 1) * 64], vp[:, h, :], rd=[ktok_r, vp_r], wr=[pu_r])
                tc, tc_r = tcs[it % 2]
                self.tt(tc, v4(pu[0:64, 0:512]), cs_, ALU.add, rd=[pu_r, cs_r_], wr=[tc_r])
                self.tt(cs_, tc, dcall[:, t, dd * 4:dd * 4 + 4].unsqueeze(2).to_broadcast([64, 4, 128]), ALU.mult,
                        rd=[tc_r, dcall_r], wr=[cs_r_])
                self.act(cb_nxt, cs_, AF.Copy, rd=[cs_r_], wr=[cb_nxt_r])
                cur = None
                if not skip_out:
                    pB, pB_r = nextS()
                    for h in range(4):
                        self.mm(pB[0:64, h * 128:(h + 1) * 128], self.selrow[:, h, :], rows[:, dd, tsl], rd=[rows_r, self.cf_r], wr=[pB_r])
                    En, En_r = Ens[it % 2]
                    self.act(En, v4(pB[0:64, 0:512]), AF.Exp, rd=[pB_r], wr=[En_r])
                    pst, pst_r = nextS()
                    for h in range(4):
                        self.mm(pst[:, h * 128:(h + 1) * 128], qk[:, 4 + h, tsl], qk[:, h, tsl], rd=[qk_r], wr=[pst_r])
                    WT, WT_r = WTs[it % 3]
                    self.tt(WT, v4(pst[:, 0:512]), mask.unsqueeze(1).to_broadcast([128, 4, 128]), ALU.mult, rd=[pst_r, self.cb_r], wr=[WT_r])
                    px, px_r = nextL()
                    for h in range(4):
                        hs = slice(h * 128, (h + 1) * 128)
                        self.mm(px[0:64, hs], vp[:, h, 0:64], WT[:, h, :], start=True, stop=False, rd=[vp_r, WT_r], wr=[px_r])
                        self.mm(px[0:64, hs], cb_cur[:, h, 0:64], qk[:, h, tsl], start=False, stop=True, rd=[cb_cur_r, qk_r], wr=[px_r])
                    py, py_r = nextL()
                    for h in range(4):
                        hs = slice(h * 128, (h + 1) * 128)
                        self.mm(py[0:64, hs], vp[:, h, 64:128], WT[:, h, :], start=True, stop=False, rd=[vp_r, WT_r], wr=[py_r])
                        self.mm(py[0:64, hs], cb_cur[:, h, 64:128], qk[:, h, tsl], start=False, stop=True, rd=[cb_cur_r, qk_r], wr=[py_r])
                    aY, aY_r = aYs[it % 2]
                    self.act(aY, v4(py[0:64, 0:512]), AF.Abs, rd=[py_r], wr=[aY_r])
                    cur = (t, px, px_r, aY, aY_r, En, En_r, it)
                if pending is not None:
                    tail(pending)
                pending = cur
        if pending is not None:
            tail(pending)
        self.dbg_dump(f"dbg_ha{l}", ha[:, :, CTXL:T], ha_r, [64, 4, SEQ], BF16)
        self.release(m2)
        wo, wo_r = self.alloc("wAo", 128, [KC, 256], BF16)
        self.load_w(wo, wo_r, d["w_in"][l][:, 768:1024], 256)
        self.post_norm_gate(l, ha, ha_r, mnw[:, l, :], wo, wo_r, 0, bA[:, l, 12:16], AF.Sigmoid, yT, yT_r)
        self.release(ms)
        return yT, yT_r, 64

    def mixer_D(self, bi, l):
        d = self.dram
        last = (l == self.NL - 1)
        oT, oT_r = self.alloc("oD", 64, [4, T], BF16)
        ms = self.mark()
        bD = self.P("bD").rearrange("p (l g) -> p l g", l=2, g=20)
        hnw = self.P("hnw").rearrange("p (l g) -> p l g", l=2, g=4)
        lbc, oml = self.lb[:, l, 0, :], self.lb[:, l, 1, :]
        qT, qT_r = self.alloc("qD", 64, [4, T], BF16)
        kT, kT_r = self.alloc("kD", 64, [4, T], BF16)
        cs, cs_r = self.alloc("csD", 64, [4, T])
        vtok, vtok_r = self.alloc("vtokD", 128, [NT, 4, 64], BF16)
        lx, lx_r = self.alloc("lxD", 64, [8])
        self.ts(lx[:, 0:4], lbc, 1e-30, ALU.add, rd=[self.lb_r], wr=[lx_r])
        self.ts(lx[:, 4:8], oml, -1.0, ALU.mult, rd=[self.lb_r], wr=[lx_r])
        m3 = self.mark()
        w, w_r = self.alloc("wDq", 128, [KC, 512], BF16, hi=True)
        self.load_w(w, w_r, d["w_in"][l][:, 2320:2832], 512, hi=True)
        vtmp, vtmp_r = self.alloc("vtmpD", 64, [4, 512], BF16)
        for b, (t0, n) in enumerate(BLOCKS):
            for h in range(4):
                ps, ps_r = self.inproj(w, w_r, h * 64, 64, b, t0, n)
                self.act(qT[:, h, t0:t0 + n], ps[0:64, 0:n], AF.Silu, rd=[ps_r, self.prm_r], wr=[qT_r], bias=bD[:, l, h:h + 1])
                ps, ps_r = self.inproj(w, w_r, (4 + h) * 64, 64, b, t0, n)
                self.act(vtmp[:, h, 0:n], ps[0:64, 0:n], AF.Identity, rd=[ps_r, self.prm_r], wr=[vtmp_r], bias=bD[:, l, 4 + h:5 + h])
            for tl in range(n // 128):
                t = t0 // 128 + tl
                pb, pb_r = self.PSB()
                for h in range(4):
                    self.tr(pb[:, h * 64:(h + 1) * 64], vtmp[:, h, tl * 128:(tl + 1) * 128], self.ident_b[0:64, 0:64], rd=[vtmp_r, self.cb_r], wr=[pb_r])
                self.cp(vtok[:, t, :, :], pb[:, 0:256].rearrange("p (h c) -> p h c", h=4, c=64), rd=[pb_r], wr=[vtok_r])
        self.release(m3, hi=True)
        for dd in range(2):
            m3 = self.mark()
            w, w_r = self.alloc("wDf", 128, [KC, 256], BF16, hi=True)
            self.load_w(w, w_r, d["w_in"][l][:, 2832 + dd * 256:3088 + dd * 256], 256, hi=True)
            rm, rm_r = self.alloc("rmD", 64, [T], BF16, hi=True)
            self.dma("sp", rm, d["rmask"][:, dd, :], wr=[rm_r])
            sbs = [self.alloc(f"sgD{i}", 64, [512]) for i in range(4)]
            for b, (t0, n) in enumerate(BLOCKS):
                for h in range(4):
                    ps, ps_r = self.inproj(w, w_r, h * 64, 64, b, t0, n)
                    sb_, sb_r = sbs[h]
                    self.act(sb_[:, 0:n], ps[0:64, 0:n], AF.Sigmoid, rd=[ps_r, self.prm_r], wr=[sb_r],
                             bias=bD[:, l, 8 + dd * 4 + h:8 + dd * 4 + h + 1])
                for h in range(4):
                    sb_, sb_r = sbs[h]
                    self.act(cs[:, h, t0:t0 + n], sb_[:, 0:n], AF.Ln, rd=[sb_r, lx_r], wr=[cs_r], scale=oml[:, h:h + 1], bias=lx[:, h:h + 1])
                    self.ts(kT[:, h, t0:t0 + n], sb_[:, 0:n], lx[:, 4 + h:5 + h], ALU.mult, s2=oml[:, h:h + 1], op1=ALU.add,
                            rd=[sb_r, lx_r, self.lb_r], wr=[kT_r])
            for h in range(4):
                if dd == 0:
                    self.scan(cs[:, h, :], rm[:, :], cs[:, h, :], 0.0, ALU.mult, ALU.subtract, rd=[cs_r, rm_r], wr=[cs_r])
                else:
                    self.scan(cs[:, h, ::-1], rm[:, ::-1], cs[:, h, ::-1], 0.0, ALU.mult, ALU.subtract, rd=[cs_r, rm_r], wr=[cs_r])
            self.release(m3, hi=True)
            m3 = self.mark()
            Sst, Sst_r = self.alloc("Sst", 64, [4, 64])
            Sbfs = [self.alloc(f"Sbf{i}", 64, [4, 64], BF16) for i in range(2)]
            self.memset(Sst[:, :, :], 0.0, wr=[Sst_r])
            self.memset(Sbfs[0][0][:, :, :], 0.0, wr=[Sbfs[0][1]])
            csref, csref_r = self.alloc("csref", 64, [4, 4])
            decs = [self.alloc(f"decD{i}", 64, [4]) for i in range(2)]
            EA = [self.alloc(f"EA{i}", 64, [4, 128]) for i in range(2)]
            EK = [self.alloc(f"EK{j}", 64, [4, 32 * (j + 1) if dd == 0 else 128 - 32 * j]) for j in range(4)]
            Ql, Ql_r = self.alloc("Ql", 64, [4, 128], BF16)
            Qgs = [self.alloc(f"Qg{i}", 64, [4, 128], BF16) for i in range(2)]
            Ke, Ke_r = self.alloc("Ke", 64, [4, 128], BF16)
            Kj = [self.alloc(f"Kj{i}", 64, [4, 128], BF16) for i in range(4)]
            for i in range(4):
                self.memset(Kj[i][0][:, :, :], 0.0, wr=[Kj[i][1]])
            ktb, ktb_r = self.alloc("ktb", 128, [256], BF16)
            WTs = [self.alloc(f"WTd{i}", 128, [4, 128], BF16) for i in range(2)]
            Stmp, Stmp_r = self.alloc("Stmp", 64, [4, 64])
            order = list(range(NT)) if dd == 0 else [1, 0] + list(range(NT - 1, 1, -1))
            mask = self.maskF if dd == 0 else self.maskB
            LBk = [(self.psf[i], self.psf_res[i]) for i in (0, 1)]
            SBk = [(self.psf[i], self.psf_res[i]) for i in (2, 3, 4, 5)]
            sbi = [0]

            def nextS():
                sbi[0] += 1
                return SBk[sbi[0] % 4]
            kr = [((0, 32 * (j + 1)) if dd == 0 else (32 * j, 128)) for j in range(4)]

            def prep(t, slot):
                skip_out = last and t < 2
                b0 = t * 128
                tsl = slice(b0, b0 + 128)
                cst = cs[:, :, tsl]
                dec, dec_r = decs[slot]
                Qg, Qg_r = Qgs[slot]
                self.memset(csref[:, :, :], 0.0, wr=[csref_r])
                if dd == 0:
                    self.cp(csref[:, :, 1:4], cs[:, :, b0 + 31:b0 + 96:32], rd=[cs_r], wr=[csref_r])
                    endc = cs[:, :, b0 + 127:b0 + 128]
                else:
                    self.cp(csref[:, :, 0:3], cs[:, :, b0 + 32:b0 + 97:32], rd=[cs_r], wr=[csref_r])
                    endc = cs[:, :, b0:b0 + 1]
                self.act(dec.rearrange("p (h o) -> p h o", h=4, o=1), endc, AF.Exp, rd=[cs_r], wr=[dec_r], scale=-1.0)
                (Eq, Eq_r), (Ee, Ee_r) = EA[0], EA[1]
                if not skip_out:
                    self.tt(Eq.rearrange("p h (j c) -> p h j c", j=4, c=32), cst.rearrange("p h (j c) -> p h j c", j=4, c=32),
                            csref.unsqueeze(3).to_broadcast([64, 4, 4, 32]), ALU.subtract, rd=[cs_r, csref_r], wr=[Eq_r])
                    for j in range(4):
                        k0, k1 = kr[j]
                        self.tt(EK[j][0], cst[:, :, k0:k1], csref[:, :, j:j + 1].to_broadcast([64, 4, k1 - k0]), ALU.subtract,
                                rd=[cs_r, csref_r], wr=[EK[j][1]], eng=("pool" if j % 2 == 0 else "dve"))
                self.tt(Ee, cst, endc.to_broadcast([64, 4, 128]), ALU.subtract, rd=[cs_r], wr=[Ee_r], eng="pool")
                if not skip_out:
                    self.act(Ql, Eq, AF.Exp, rd=[Eq_r], wr=[Ql_r], scale=-1.0)
                    self.act(Qg, cst, AF.Exp, rd=[cs_r], wr=[Qg_r], scale=-1.0)
                    for j in range(4):
                        k0, k1 = kr[j]
                        self.act(Kj[j][0][:, :, k0:k1], EK[j][0], AF.Exp, rd=[EK[j][1]], wr=[Kj[j][1]])
                self.act(Ke, Ee, AF.Exp, rd=[Ee_r], wr=[Ke_r])
                if not skip_out:
                    self.tt(Ql, Ql, qT[:, :, tsl], ALU.mult, rd=[Ql_r, qT_r], wr=[Ql_r])
                    self.tt(Qg, Qg, qT[:, :, tsl], ALU.mult, rd=[Qg_r, qT_r], wr=[Qg_r])
                    for j in range(4):
                        k0, k1 = kr[j]
                        self.stt(Kj[j][0][:, :, k0:k1], Kj[j][0][:, :, k0:k1], 1.0e26, kT[:, :, b0 + k0:b0 + k1], ALU.min, ALU.mult,
                                 rd=[Kj[j][1], kT_r], wr=[Kj[j][1]])
                self.tt(Ke, Ke, kT[:, :, tsl], ALU.mult, rd=[Ke_r, kT_r], wr=[Ke_r])
                pb, pb_r = self.PSB()
                for h in range(4):
                    self.tr(pb[:, h * 64:(h + 1) * 64], Ke[:, h, :], self.ident_b[0:64, 0:64], rd=[Ke_r, self.cb_r], wr=[pb_r])
                self.act(ktb, pb[:, 0:256], AF.Copy, rd=[pb_r], wr=[ktb_r])
                ps3, ps3_r = LBk[slot]
                for h in range(4):
                    self.mm(ps3[0:64, h * 64:(h + 1) * 64], ktb[:, h * 64:(h + 1) * 64], vtok[:, t, h, :], rd=[ktb_r, vtok_r], wr=[ps3_r])
                if not skip_out:
                    ps, ps_r = nextS()
                    for h in range(4):
                        for j in range(4):
                            self.mm(ps[:, h * 128 + j * 32:h * 128 + (j + 1) * 32], Kj[j][0][:, h, :], Ql[:, h, j * 32:(j + 1) * 32],
                                    rd=[Kj[j][1], Ql_r], wr=[ps_r])
                    WT, WT_r = WTs[slot]
                    self.tt(WT, ps[:, 0:512].rearrange("p (h t) -> p h t", h=4, t=128), mask.unsqueeze(1).to_broadcast([128, 4, 128]),
                            ALU.mult, rd=[ps_r, self.cb_r], wr=[WT_r])

            def chain(t, slot, k):
                skip_out = last and t < 2
                tsl = slice(t * 128, (t + 1) * 128)
                dec, dec_r = decs[slot]
                Qg, Qg_r = Qgs[slot]
                ps3, ps3_r = LBk[slot]
                cur, cur_r = Sbfs[k % 2]
                nxt, nxt_r = Sbfs[(k + 1) % 2]
                self.tt(Stmp, Sst, dec.unsqueeze(2).to_broadcast([64, 4, 64]), ALU.mult, rd=[Sst_r, dec_r], wr=[Stmp_r])
                self.tt(Sst, Stmp, ps3[0:64, 0:256].rearrange("p (h c) -> p h c", h=4, c=64), ALU.add, rd=[ps3_r, Stmp_r], wr=[Sst_r])
                self.act(nxt, Sst, AF.Copy, rd=[Sst_r], wr=[nxt_r])
                if not skip_out:
                    WT, WT_r = WTs[slot]
                    ps2, ps2_r = nextS()
                    for h in range(4):
                        hs = slice(h * 128, (h + 1) * 128)
                        self.mm(ps2[0:64, hs], vtok[:, t, h, :], WT[:, h, :], start=True, stop=False, rd=[vtok_r, WT_r], wr=[ps2_r])
                        self.mm(ps2[0:64, hs], cur[:, h, :], Qg[:, h, :], start=False, stop=True, rd=[cur_r, Qg_r], wr=[ps2_r])
                    p2v = ps2[0:64, 0:512].rearrange("p (h t) -> p h t", h=4, t=128)
                    if dd == 0:
                        self.act(oT[:, :, tsl], p2v, AF.Copy, rd=[ps2_r], wr=[oT_r.sub(t)])
                    else:
                        self.tt(oT[:, :, tsl], p2v, oT[:, :, tsl], ALU.add, rd=[ps2_r, oT_r.sub(t)], wr=[oT_r.sub(t)])
            prep(order[0], 0)
            for i, t in enumerate(order):
                if i + 1 < NT:
                    prep(order[i + 1], (i + 1) % 2)
                chain(t, i % 2, i)
            self.release(m3)
        self.dbg_dump(f"dbg_hd{l}", oT[:, :, CTXL:T], oT_r, [64, 4, SEQ], BF16)
        self.release(ms)
        wg, wg_r = self.alloc("wDg", 128, [KC, 256], BF16)
        self.load_w(wg, wg_r, d["w_in"][l][:, 3344:3600], 256)
        self.post_norm_gate(l, oT, oT_r, hnw[:, l, :], wg, wg_r, 0, bD[:, l, 16:20], AF.Silu, oT, oT_r)
        self.release(ms)
        return oT, oT_r, 64

    def wrap_pi(self, a, a_r, tmp, tmp_r, n):
        self.ts(tmp[:, 0:n], a[:, 0:n], PI, ALU.is_gt, rd=[a_r], wr=[tmp_r])
        self.stt(a[:, 0:n], tmp[:, 0:n], -2.0 * PI, a[:, 0:n], ALU.mult, ALU.add, rd=[tmp_r, a_r], wr=[a_r])
        self.ts(tmp[:, 0:n], a[:, 0:n], -PI, ALU.is_lt, rd=[a_r], wr=[tmp_r])
        self.stt(a[:, 0:n], tmp[:, 0:n], 2.0 * PI, a[:, 0:n], ALU.mult, ALU.add, rd=[tmp_r, a_r], wr=[a_r])
        self.ts(a[:, 0:n], a[:, 0:n], -3.141592, ALU.max, s2=3.141592, op1=ALU.min, rd=[a_r], wr=[a_r])

    def mixer_B(self, bi, l):
        d = self.dram
        last = (l == self.NL - 1)
        yT, yT_r = self.alloc("yB", 128, [2, T], BF16)
        ms = self.mark()
        x0c, x0c_r = self.alloc("x0c", 128, [2, T], BF16)
        uT, uT_r = self.alloc("uTB", 128, [2, T], BF16)
        m3 = self.mark()
        w, w_r = self.alloc("wB", 128, [KC, 768], BF16, hi=True)
        self.load_w(w, w_r, d["w_in"][l][:, 1040:1808], 768, hi=True)
        x1c, x1c_r = self.alloc("x1c", 128, [2, T])
        praw, praw_r = self.alloc("praw", 128, [T])
        pcv, pcv_r = self.alloc("pcv", 128, [T])
        hsw = self.P("hsw").rearrange("p (l j c) -> p l j c", l=2, j=3, c=6)
        hsb = self.P("hsb").rearrange("p (l c) -> p l c", l=2, c=6)
        bB = self.P("bB").rearrange("p (l c) -> p l c", l=2, c=6)
        seqs = [(CTXL, SEQ, "L")] if last else [(0, CTXL, "S"), (CTXL, SEQ, "L")]
        lo = CTXL if last else 0
        for c in range(6):
            for b, (t0, n) in enumerate(BLOCKS):
                if b == 0 and last:
                    continue
                ps, ps_r = self.inproj(w, w_r, c * 128, 128, b, t0, n)
                self.act(praw[:, t0:t0 + n], ps[:, 0:n], AF.Identity, rd=[ps_r, self.prm_r], wr=[praw_r], bias=bB[:, l, c:c + 1])
            for (s0, Ls, _) in seqs:
                e = s0 + Ls
                self.ts(pcv[:, s0:e], praw[:, s0:e], hsw[:, l, 1, c:c + 1], ALU.mult, s2=hsb[:, l, c:c + 1], op1=ALU.add,
                        rd=[praw_r, self.prm_r], wr=[pcv_r])
                self.stt(pcv[:, s0 + 1:e], praw[:, s0:e - 1], hsw[:, l, 0, c:c + 1], pcv[:, s0 + 1:e], ALU.mult, ALU.add,
                         rd=[praw_r, self.prm_r, pcv_r], wr=[pcv_r])
                self.stt(pcv[:, s0:e - 1], praw[:, s0 + 1:e], hsw[:, l, 2, c:c + 1], pcv[:, s0:e - 1], ALU.mult, ALU.add,
                         rd=[praw_r, self.prm_r, pcv_r], wr=[pcv_r])
            if c < 2:
                self.act(x0c[:, c, lo:T], pcv[:, lo:T], AF.Copy, rd=[pcv_r], wr=[x0c_r])
            elif c < 4:
                self.cp(x1c[:, c - 2, lo:T], pcv[:, lo:T], rd=[pcv_r], wr=[x1c_r], eng="pool")
            else:
                self.tt(uT[:, c - 4, lo:T], pcv[:, lo:T], x1c[:, c - 4, lo:T], ALU.mult, rd=[pcv_r, x1c_r], wr=[uT_r])
        self.release(m3, hi=True)
        for (s0, Ls, tag) in seqs:
            self.hy_conv(l, s0, Ls, tag, x0c, x0c_r, uT, uT_r, yT, yT_r)
        self.release(ms)
        return yT, yT_r, 128

    def hy_conv(self, l, s0, L, tag, x0c, x0c_r, uT, uT_r, yT, yT_r):
        d = self.dram
        mc = L // 128
        m0 = self.mark()
        RH, RH_r = self.alloc("RH", 128, [mc, 768], BF16)
        YS, YS_r = self.alloc("YS", 128, [2 * mc, 256], BF16)
        m1 = self.mark()
        zt, zt_r = self.alloc("zt", 17, [L])
        self.dma("sp", zt, d["z" + tag], wr=[zt_r])
        hd2, hd2_r = self.alloc("hd2", 64, [L])
        arg, arg_r = self.alloc("harg", 64, [512])
        tmpm, tmpm_r = self.alloc("htmp", 64, [512])
        hd1, hd1_r = self.alloc("hd1", 64, [512])
        fb, fb_r = self.alloc("hfb", 64, [2])
        f1 = self.P("hy_freq1")[:, l:l + 1]
        f2 = self.P("hy_freq2")[:, l:l + 1]
        self.tt(fb[:, 0:1], f1, self.P("hy_b1")[:, l:l + 1], ALU.mult, rd=[self.prm_r], wr=[fb_r])
        self.tt(fb[:, 1:2], f2, self.P("hy_b2")[:, l:l + 1], ALU.mult, rd=[self.prm_r], wr=[fb_r])
        hw1 = self.P("hw1").rearrange("p (l c) -> p l c", l=2, c=64)
        hw2 = self.P("hw2").rearrange("p (l c) -> p l c", l=2, c=64)
        hw3 = self.P("hw3").rearrange("p (l c) -> p l c", l=2, c=512)
        nb = min(512, L)
        for c0 in range(0, L, nb):
            ps, ps_r = self.PS()
            self.mm(ps[0:64, 0:nb], hw1[:, l, :], zt[:, c0:c0 + nb], rd=[self.prm_r, zt_r], wr=[ps_r])
            self.act(arg[:, 0:nb], ps[0:64, 0:nb], AF.Identity, rd=[ps_r, self.prm_r, fb_r], wr=[arg_r], bias=fb[:, 0:1], scale=f1)
            self.wrap_pi(arg, arg_r, tmpm, tmpm_r, nb)
            self.act(hd1[:, 0:nb], arg[:, 0:nb], AF.Sin, rd=[arg_r], wr=[hd1_r])
            ps, ps_r = self.PS()
            self.mm(ps[0:64, 0:nb], hw2[:, l, :], hd1[:, 0:nb], rd=[self.prm_r, hd1_r], wr=[ps_r])
            self.act(arg[:, 0:nb], ps[0:64, 0:nb], AF.Identity, rd=[ps_r, self.prm_r, fb_r], wr=[arg_r], bias=fb[:, 1:2], scale=f2)
            self.wrap_pi(arg, arg_r, tmpm, tmpm_r, nb)
            self.act(hd2[:, c0:c0 + nb], arg[:, 0:nb], AF.Sin, rd=[arg_r], wr=[hd2_r])
        dcs = [self.alloc(f"hdc{i}", 128, [2, 256]) for i in range(2)]
        hfs = [self.alloc(f"hf{i}", 128, [256]) for i in range(2)]
        hbs = [self.alloc(f"hb{i}", 128, [256]) for i in range(2)]
        for m in range(mc):
            dc, dc_r = dcs[m % 2]
            self.dma("sp", dc, d["dec" + tag][:, m], wr=[dc_r])
            ps, ps_r = self.PS()
            self.mm(ps[:, 0:512], hd2[:, m * 128:(m + 1) * 128], hw3[:, l, :], rd=[hd2_r, self.prm_r], wr=[ps_r])
            hf, hf_r = hfs[m % 2]
            hb, hb_r = hbs[m % 2]
            self.tt(hf, ps[:, 0:256], dc[:, 0, :], ALU.mult, rd=[ps_r, dc_r], wr=[hf_r])
            self.tt(hb, ps[:, 256:512], dc[:, 1, :], ALU.mult, rd=[ps_r, dc_r], wr=[hb_r])
            self.tt(RH[:, m, 0:256], hf, hb, ALU.add, rd=[hf_r, hb_r], wr=[RH_r.sub(m)], eng="pool")
            self.tt(RH[:, m, 512:768], hb, hf, ALU.subtract, rd=[hf_r, hb_r], wr=[RH_r.sub(m)], eng="pool")
            pb, pb_r = self.PSB()
            for cc in range(2):
                tk = s0 + m * 128
                self.tr(pb[:, cc * 128:(cc + 1) * 128], uT[:, cc, tk:tk + 128], self.ident_b, rd=[uT_r, self.cb_r], wr=[pb_r])
            self.cp(RH[:, m, 256:512], pb[:, 0:256], rd=[pb_r], wr=[RH_r.sub(m)])
        self.release(m1)
        m1 = self.mark()
        Fts = [self.alloc(f"Ft{i}", 128, [mc, 128], BF16) for i in range(4)]
        Kcs = [self.alloc(f"Kc{i}", 128, [256]) for i in range(2)]
        Kds = [self.alloc(f"Kd{i}", 128, [256]) for i in range(2)]
        Pt = [self.alloc(f"Pp{i}", 128, [256]) for i in range(4)]
        kny, kny_r = self.alloc("kny", 1, [256])
        for i in range(mc):
            fa, fa_r = Fts[(2 * i) % 4]
            fs, fs_r = Fts[(2 * i + 1) % 4]
            self.dma("sp", fa, d["F" + tag][i], wr=[fa_r])
            self.dma("sp", fs, d["F" + tag][mc + i], wr=[fs_r])
            pc, pc_r = self.PS()
            for m in range(mc):
                self.mm(pc[:, 0:512], fa[:, m, :], RH[:, m, 0:512], start=(m == 0), stop=(m == mc - 1), rd=[fa_r, RH_r], wr=[pc_r])
            pS, pS_r = self.PS()
            for m in range(mc):
                self.mm(pS[:, 0:512], fs[:, m, :], RH[:, m, 256:768], start=(m == 0), stop=(m == mc - 1), rd=[fs_r, RH_r], wr=[pS_r])
            if i == 0:
                pn, pn_r = self.PS()
                for m in range(mc):
                    self.mm(pn[:, 0:256], fs[:, m, :], RH[:, m, 0:256], start=(m == 0), stop=(m == mc - 1), rd=[fs_r, RH_r], wr=[pn_r])
            Kc, Kc_r = Kcs[i % 2]
            Kd, Kd_r = Kds[i % 2]
            self.act(Kc, pc[:, 0:256], AF.Copy, rd=[pc_r], wr=[Kc_r])
            self.act(Kd, pS[:, 256:512], AF.Copy, rd=[pS_r], wr=[Kd_r])
            (P1, P1_r), (P2, P2_r), (P3, P3_r), (P4, P4_r) = Pt
            self.tt(P1, pc[:, 256:512], Kc, ALU.mult, rd=[pc_r, Kc_r], wr=[P1_r])
            self.tt(P2, pS[:, 0:256], Kd, ALU.mult, rd=[pS_r, Kd_r], wr=[P2_r])
            self.tt(YS[:, i, :], P1, P2, ALU.add, rd=[P1_r, P2_r], wr=[YS_r.sub(i)], eng="pool")
            self.tt(P3, pc[:, 256:512], Kd, ALU.mult, rd=[pc_r, Kd_r], wr=[P3_r])
            self.tt(P4, pS[:, 0:256], Kc, ALU.mult, rd=[pS_r, Kc_r], wr=[P4_r])
            self.tt(YS[:, mc + i, :], P3, P4, ALU.subtract, rd=[P3_r, P4_r], wr=[YS_r.sub(mc + i)], eng="pool")
            if i == 0:
                self.cp(YS[0:1, 0, :], P1[0:1, :], rd=[P1_r, YS_r.sub(0)], wr=[YS_r.sub(0)], eng="pool")
                self.act(kny, pn[0:1, 0:256], AF.Copy, rd=[pn_r], wr=[kny_r])
                self.tt(YS[0:1, mc, :], pS[0:1, 0:256], kny, ALU.mult, rd=[pS_r, kny_r, YS_r.sub(mc)], wr=[YS_r.sub(mc)])
        self.release(m1)
        m1 = self.mark()
        n = min(512, L)
        q = min(8, 2 * mc)
        Gts = [self.alloc(f"Gt{i}", 128, [q, n], BF16) for i in range(3)]
        t1s = [self.alloc(f"ht1{i}", 128, [n]) for i in range(2)]
        dsk = self.P("hskip").rearrange("p (l c) -> p l c", l=2, c=2)
        gi = 0
        for tb in range(L // n):
            p0, p0_r = self.PS()
            p1, p1_r = self.PS()
            for qi in range(2 * mc // q):
                g, g_r = Gts[gi % 3]
                gi += 1
                self.dma("sp", g, d["G" + tag][tb][:, qi * q:(qi + 1) * q, :], wr=[g_r])
                for r in range(q):
                    fc = qi * q + r
                    self.mm(p0[:, 0:n], YS[:, fc, 0:128], g[:, r, :], start=(fc == 0), stop=(fc == 2 * mc - 1), rd=[YS_r, g_r], wr=[p0_r])
                    self.mm(p1[:, 0:n], YS[:, fc, 128:256], g[:, r, :], start=(fc == 0), stop=(fc == 2 * mc - 1), rd=[YS_r, g_r], wr=[p1_r])
            tk = s0 + tb * n
            for cc, (p, p_r) in enumerate(((p0, p0_r), (p1, p1_r))):
                t1, t1_r = t1s[cc]
                self.stt(t1[:, 0:n], uT[:, cc, tk:tk + n], dsk[:, l, cc:cc + 1], p[:, 0:n], ALU.mult, ALU.add, rd=[uT_r, self.prm_r, p_r], wr=[t1_r])
                self.tt(yT[:, cc, tk:tk + n], t1[:, 0:n], x0c[:, cc, tk:tk + n], ALU.mult, rd=[t1_r, x0c_r], wr=[yT_r])
        self.release(m0)


_PROG_CACHE = {}


def _get_prog(NB, NL, stages, dbg, prm_off, nprm):
    key = (NB, NL, stages, dbg)
    if key not in _PROG_CACHE:
        p = Prog(NB, NL, stages, dbg)
        off = dict(prm_off)
        off["__n__"] = nprm
        p.build(off, nprm)
        _PROG_CACHE[key] = p
    return _PROG_CACHE[key]


def make_in_maps(inp, n_cores, NB):
    P = _prep_shared(inp)
    prm = P.build()
    cst = _constants()
    f32 = lambda a: np.ascontiguousarray(np.asarray(a, np.float32))
    shared = {
        "prm": prm,
        "ada_w": f32(inp["ada_w"]), "w_in": f32(inp["w_in"]), "w_gate": f32(inp["w_gate"]),
        "w_branch": f32(inp["w_branch"]), "w_out": f32(inp["w_out"]), "w_ffn_in": f32(inp["w_ffn_in"]),
        "w_ffn_out": f32(inp["w_ffn_out"]),
        "gwsT": f32(np.transpose(np.asarray(inp["gm_ws"], np.float32), (3, 0, 1, 2))),
        "gbs": f32(np.asarray(inp["gm_bs"], np.float32).reshape(1, -1)),
    }
    for k, v in cst.items():
        shared[k] = v
    maps = []
    x = np.asarray(inp["x"], np.float32)
    ctx = np.asarray(inp["ctx"], np.float32)
    c = np.asarray(inp["c"], np.float32)
    cc = np.asarray(inp["c_ctx"], np.float32)
    for i in range(n_cores):
        b0 = i * NB
        m = dict(shared)
        m["xin"] = np.ascontiguousarray(x[b0:b0 + NB])
        m["cin"] = np.ascontiguousarray(ctx[b0:b0 + NB])
        cols = [_col(c[b0 + (j % NB)]) for j in range(2)] + [_col(cc)]
        m["csT"] = np.ascontiguousarray(np.stack(cols, axis=2))
        maps.append(m)
    return maps, P.off, prm.shape[1]


def kernel(**inp):
    n_cores, NB = 8, 2
    maps, off, nprm = make_in_maps(inp, n_cores, NB)
    prog = _get_prog(NB, 2, "ABCD", False, off, nprm)
    res = run_bass_kernel_spmd(prog.nc, maps, core_ids=list(range(n_cores)))
    out = np.concatenate([np.asarray(r["yout"], np.float32) for r in res.results], axis=0)
    return out
```
